# Optimizing a Trainium2 kernel written in Bass

```python
import jax, jax.numpy as jnp
from jax import lax
import numpy as np

D_MODEL = 1024
BATCH = 8
SEQ = 4096
DEPTH = 2

CHUNK = 64
QBLOCK = 128
EPS = 1e-6
N_BRANCH = 4

A_HEADS = 8
A_NOPE = 64
A_ROPE = 32
A_VDIM = 64
A_QRANK = D_MODEL // 4
A_KVRANK = D_MODEL // 8
A_WIDTH = A_HEADS * A_VDIM
ROPE_THETA = 10000.0

B_HEADS = 4
B_QK = 64
B_V = 128
B_WIDTH = B_HEADS * B_V
MLSTM_CHUNK = CHUNK
F_BIAS_INIT = 3.0

C_WIDTH = D_MODEL // 2
CONV_K = 3

D_GROUPS = 4
D_POS = 128
D_WIDTH = D_MODEL // 2
D_GCH = D_WIDTH // D_GROUPS

IN_SIZES = (A_QRANK, A_KVRANK, A_ROPE, A_WIDTH,
            B_HEADS * B_QK, B_HEADS * B_QK, B_WIDTH, B_HEADS, B_HEADS, B_WIDTH, B_WIDTH,
            C_WIDTH, C_WIDTH, C_WIDTH, C_WIDTH,
            2 * D_WIDTH, D_WIDTH)
IN_DIM = (A_QRANK + A_KVRANK + A_ROPE + A_WIDTH
          + 2 * B_HEADS * B_QK + 3 * B_WIDTH + 2 * B_HEADS
          + 4 * C_WIDTH + 3 * D_WIDTH)

kernel_name = 'hybrid_gated_parallel_mixers'


def split_cols(z):
    idx = []
    acc = 0
    for s in IN_SIZES[:-1]:
        acc += s
        idx.append(acc)
    return jnp.split(z, idx, axis=-1)


def rmsnorm(x, g):
    xf = x.astype(jnp.float32)
    y = xf * lax.rsqrt(jnp.mean(xf * xf, axis=-1, keepdims=True) + EPS)
    return (y * g.astype(jnp.float32)).astype(x.dtype)


def layernorm(x, g):
    xf = x.astype(jnp.float32)
    xc = xf - jnp.mean(xf, axis=-1, keepdims=True)
    y = xc * lax.rsqrt(jnp.mean(xc * xc, axis=-1, keepdims=True) + EPS)
    return (y * g.astype(jnp.float32)).astype(x.dtype)


def rope(t, pos):
    half = t.shape[-1] // 2
    inv = ROPE_THETA ** (-jnp.arange(half, dtype=jnp.float32) / half)
    ang = pos.astype(jnp.float32)[..., None] * inv
    cos = jnp.cos(ang).astype(t.dtype)
    sin = jnp.sin(ang).astype(t.dtype)
    t1, t2 = t[..., :half], t[..., half:]
    return jnp.concatenate([t1 * cos - t2 * sin, t1 * sin + t2 * cos], axis=-1)


def mla_branch(cq, ckv, krope, pos, g_cq, g_ckv, w_uq, w_ukv):
    bsz, s_len, _ = cq.shape
    q = (rmsnorm(cq, g_cq) @ w_uq).reshape(bsz, s_len, A_HEADS, A_NOPE + A_ROPE).transpose(0, 2, 1, 3)
    kv = (rmsnorm(ckv, g_ckv) @ w_ukv).reshape(bsz, s_len, A_HEADS, A_NOPE + A_VDIM).transpose(0, 2, 1, 3)
    k_nope, v = kv[..., :A_NOPE], kv[..., A_NOPE:]
    pos_h = pos[:, None, :]
    q_rope = rope(q[..., A_NOPE:], pos_h)
    k_rope = rope(krope[:, None], pos_h)
    q = jnp.concatenate([q[..., :A_NOPE], q_rope], axis=-1)
    k = jnp.concatenate([k_nope, jnp.broadcast_to(k_rope, k_nope.shape[:-1] + (A_ROPE,))], axis=-1)
    scale = (A_NOPE + A_ROPE) ** -0.5
    chunk_id = jnp.arange(s_len) // CHUNK
    outs = []
    for qb in range(s_len // QBLOCK):
        lo, hi = qb * QBLOCK, (qb + 1) * QBLOCK
        sc = jnp.einsum('bhqd,bhkd->bhqk', q[:, :, lo:hi], k[:, :, :hi]).astype(jnp.float32) * scale
        mask = chunk_id[None, :hi] <= chunk_id[lo:hi, None]
        sc = jnp.where(mask, sc, -1e30)
        p = jax.nn.softmax(sc, axis=-1).astype(v.dtype)
        outs.append(jnp.einsum('bhqk,bhkd->bhqd', p, v[:, :, :hi]))
    o = jnp.concatenate(outs, axis=2)
    return o.transpose(0, 2, 1, 3).reshape(bsz, s_len, A_WIDTH)


def mlstm_branch(q, k, v, i_pre, f_pre, o_pre, f_bias, g_mh):
    bsz, s_len, _ = q.shape
    L = MLSTM_CHUNK
    nc = s_len // L
    f32 = jnp.float32

    def heads(t, d):
        return t.reshape(bsz, nc, L, B_HEADS, d).transpose(0, 3, 1, 2, 4).astype(f32)

    def gates(t):
        return t.reshape(bsz, nc, L, B_HEADS).transpose(0, 3, 1, 2).astype(f32)

    qh = heads(q, B_QK) * (B_QK ** -0.5)
    kh = heads(k, B_QK)
    vh = heads(v, B_V)
    ig = gates(i_pre)
    lf = jax.nn.log_sigmoid(gates(f_pre + f_bias))
    b = jnp.cumsum(lf, axis=-1)
    g = b[..., -1]
    a = g[..., None] - b + ig
    m_loc = jnp.max(a, axis=-1)
    w = jnp.exp(a - m_loc[..., None])
    ck = jnp.einsum('bhcl,bhcld,bhcle->bhcde', w, kh, vh)
    nk = jnp.einsum('bhcl,bhcld->bhcd', w, kh)

    def step(carry, inp):
        c_st, n_st, m_st = carry
        ck_c, nk_c, g_c, ml_c = inp
        m_new = jnp.maximum(g_c + m_st, ml_c)
        s_old = jnp.exp(g_c + m_st - m_new)
        s_new = jnp.exp(ml_c - m_new)
        c_new = s_old[..., None, None] * c_st + s_new[..., None, None] * ck_c
        n_new = s_old[..., None] * n_st + s_new[..., None] * nk_c
        return (c_new, n_new, m_new), (c_st, n_st, m_st)

    init = (jnp.zeros((bsz, B_HEADS, B_QK, B_V), f32),
            jnp.zeros((bsz, B_HEADS, B_QK), f32),
            jnp.zeros((bsz, B_HEADS), f32))
    xs = (jnp.moveaxis(ck, 2, 0), jnp.moveaxis(nk, 2, 0), jnp.moveaxis(g, 2, 0), jnp.moveaxis(m_loc, 2, 0))
    _, (c_prev, n_prev, m_prev) = lax.scan(step, init, xs)
    c_prev = jnp.moveaxis(c_prev, 0, 2)
    n_prev = jnp.moveaxis(n_prev, 0, 2)
    m_prev = jnp.moveaxis(m_prev, 0, 2)

    causal = jnp.tril(jnp.ones((L, L), dtype=bool))
    dmat = jnp.where(causal, b[..., :, None] - b[..., None, :] + ig[..., None, :], -jnp.inf)
    inter = b + m_prev[..., None]
    m_t = jnp.maximum(jnp.max(dmat, axis=-1), inter)
    pq = jnp.exp(dmat - m_t[..., None]) * jnp.einsum('bhctd,bhcsd->bhcts', qh, kh)
    e_inter = jnp.exp(inter - m_t)
    num = jnp.einsum('bhcts,bhcse->bhcte', pq, vh) + e_inter[..., None] * jnp.einsum('bhctd,bhcde->bhcte', qh, c_prev)
    den = jnp.sum(pq, axis=-1) + e_inter * jnp.einsum('bhctd,bhcd->bhct', qh, n_prev)
    h = num / jnp.maximum(jnp.abs(den), jnp.exp(-m_t))[..., None]
    h = h * lax.rsqrt(jnp.mean(h * h, axis=-1, keepdims=True) + EPS)
    h = h.transpose(0, 2, 3, 1, 4).reshape(bsz, s_len, B_WIDTH) * g_mh.astype(f32)
    return (h * jax.nn.sigmoid(o_pre.astype(f32))).astype(v.dtype)


def shortconv_branch(xc, bg, cg, w_conv, b_conv):
    u = cg * xc
    y = lax.conv_general_dilated(u, w_conv, window_strides=(1,), padding=[(CONV_K - 1, 0)],
                                 dimension_numbers=('NWC', 'WIO', 'NWC'),
                                 feature_group_count=C_WIDTH)
    return bg * (y + b_conv)


def sgu_branch(uv, g_sv, w_s, b_s):
    bsz, s_len, _ = uv.shape
    uv = jax.nn.gelu(uv)
    u, v = uv[..., :D_WIDTH], uv[..., D_WIDTH:]
    v = layernorm(v, g_sv).reshape(bsz, s_len // D_POS, D_POS, D_GROUPS, D_GCH)
    ws = jnp.tril(w_s)
    mixed = jnp.einsum('gts,bnsgc->bntgc', ws, v) + b_s.T[None, None, :, :, None]
    return u * mixed.reshape(bsz, s_len, D_WIDTH)


def hybrid_layer(x, pos, g_pre, g_post, w_in, w_gate, b_gate, g_cq, g_ckv, w_uq, w_ukv,
                 f_bias, g_mh, w_conv, b_conv, g_sv, w_s, b_s, w_pa, w_pb, w_pc, w_pd, w_out):
    bsz, s_len, _ = x.shape
    h = rmsnorm(x, g_pre)
    (cq, ckv, krope, ga,
     mq, mk, mv, mi, mf, mo, gb,
     cx, cb, cc, gc,
     duv, gd) = split_cols(h @ w_in)
    y_a = mla_branch(cq, ckv, krope, pos, g_cq, g_ckv, w_uq, w_ukv) * jax.nn.silu(ga)
    y_b = mlstm_branch(mq, mk, mv, mi, mf, mo, f_bias, g_mh) * jax.nn.silu(gb)
    y_c = shortconv_branch(cx, cb, cc, w_conv, b_conv) * jax.nn.silu(gc)
    y_d = sgu_branch(duv, g_sv, w_s, b_s) * jax.nn.silu(gd)
    gates = jax.nn.sigmoid((h @ w_gate + b_gate).astype(jnp.float32)).astype(x.dtype)
    gates = gates.reshape(bsz, s_len, N_BRANCH, D_MODEL)
    merged = (gates[:, :, 0] * (y_a @ w_pa) + gates[:, :, 1] * (y_b @ w_pb)
              + gates[:, :, 2] * (y_c @ w_pc) + gates[:, :, 3] * (y_d @ w_pd))
    return x + rmsnorm(merged @ w_out, g_post)


def setup_inputs(seed: int = 0) -> dict:
    key = jax.random.key(seed)
    ks = jax.random.split(key, 32)
    f32 = jnp.float32

    def nrm(k, shape, fan_in):
        return jax.random.normal(k, shape, f32) * (fan_in ** -0.5)

    def gain(k, shape):
        return 1.0 + 0.05 * jax.random.normal(k, shape, f32)

    x = jax.random.normal(ks[0], (BATCH, SEQ, D_MODEL), f32)
    offsets = jax.random.randint(ks[1], (BATCH, 1), 0, 64) * CHUNK
    positions = (offsets + jnp.arange(SEQ)[None, :]).astype(jnp.int32)
    return {
        'x': x,
        'positions': positions,
        'g_pre': gain(ks[2], (DEPTH, D_MODEL)),
        'g_post': gain(ks[3], (DEPTH, D_MODEL)),
        'w_in': nrm(ks[4], (DEPTH, D_MODEL, IN_DIM), D_MODEL),
        'w_gate': nrm(ks[5], (DEPTH, D_MODEL, N_BRANCH * D_MODEL), D_MODEL),
        'b_gate': 0.01 * jax.random.normal(ks[6], (DEPTH, N_BRANCH * D_MODEL), f32),
        'g_cq': gain(ks[7], (DEPTH, A_QRANK)),
        'g_ckv': gain(ks[8], (DEPTH, A_KVRANK)),
        'w_uq': nrm(ks[9], (DEPTH, A_QRANK, A_HEADS * (A_NOPE + A_ROPE)), A_QRANK),
        'w_ukv': nrm(ks[10], (DEPTH, A_KVRANK, A_HEADS * (A_NOPE + A_VDIM)), A_KVRANK),
        'f_bias': F_BIAS_INIT + 0.1 * jax.random.normal(ks[11], (DEPTH, B_HEADS), f32),
        'g_mh': gain(ks[12], (DEPTH, B_WIDTH)),
        'w_conv': nrm(ks[13], (DEPTH, CONV_K, 1, C_WIDTH), CONV_K),
        'b_conv': 0.01 * jax.random.normal(ks[14], (DEPTH, C_WIDTH), f32),
        'g_sv': gain(ks[15], (DEPTH, D_WIDTH)),
        'w_s': nrm(ks[16], (DEPTH, D_GROUPS, D_POS, D_POS), D_POS),
        'b_s': 1.0 + 0.01 * jax.random.normal(ks[17], (DEPTH, D_GROUPS, D_POS), f32),
        'w_pa': nrm(ks[18], (DEPTH, A_WIDTH, D_MODEL), A_WIDTH),
        'w_pb': nrm(ks[19], (DEPTH, B_WIDTH, D_MODEL), B_WIDTH),
        'w_pc': nrm(ks[20], (DEPTH, C_WIDTH, D_MODEL), C_WIDTH),
        'w_pd': nrm(ks[21], (DEPTH, D_WIDTH, D_MODEL), D_WIDTH),
        'w_out': nrm(ks[22], (DEPTH, D_MODEL, D_MODEL), D_MODEL),
    }


def reference(x, positions, g_pre, g_post, w_in, w_gate, b_gate, g_cq, g_ckv, w_uq, w_ukv,
              f_bias, g_mh, w_conv, b_conv, g_sv, w_s, b_s, w_pa, w_pb, w_pc, w_pd, w_out):
    for l in range(DEPTH):
        x = hybrid_layer(x, positions, g_pre[l], g_post[l], w_in[l], w_gate[l], b_gate[l],
                         g_cq[l], g_ckv[l], w_uq[l], w_ukv[l], f_bias[l], g_mh[l],
                         w_conv[l], b_conv[l], g_sv[l], w_s[l], b_s[l],
                         w_pa[l], w_pb[l], w_pc[l], w_pd[l], w_out[l])
    return x
```

```python
import numpy as np
import concourse.bass as bass
import concourse.mybir as mybir
from concourse.bass_utils import run_bass_kernel_spmd

F32 = mybir.dt.float32
BF16 = mybir.dt.bfloat16
I32 = mybir.dt.int32
AF = mybir.ActivationFunctionType
ALU = mybir.AluOpType
AX = mybir.AxisListType


class Buf:
    __slots__ = ("name", "t", "last_w", "readers")

    def __init__(self, name, t=None):
        self.name = name
        self.t = t
        self.last_w = None
        self.readers = []


class Op:
    __slots__ = ("eng", "fn", "deps", "is_dma", "ndma", "semkey", "signal", "sem", "val", "gidx")

    def __init__(self, eng, fn, is_dma=False, ndma=1, semkey=None):
        self.eng = eng
        self.fn = fn
        self.deps = []
        self.is_dma = is_dma
        self.ndma = ndma
        self.semkey = semkey
        self.signal = False
        self.sem = None
        self.val = 0
        self.gidx = 0


class Planner:
    ENGS = ("pe", "act", "dve", "pool", "sp")
    SEM_EPOCH = 30000

    def __init__(self, nc):
        self.nc = nc
        self.ops = {e: [] for e in self.ENGS}
        self.all_ops = []
        self.last_dma = {}
        self.fence = []
        self.sb_off = int(nc.sbuf_base)
        self.sb_hi = 0
        self.sb_top = int(nc.sbuf_top)
        self.top_off = self.sb_top
        self.n_names = 0
        self.psum_bufs = []
        self.sbuf_base = None

    def _dsize(self, dtype):
        return {F32: 4, BF16: 2, I32: 4}[dtype]

    def sbuf(self, name, shape, dtype, align=64, top=False):
        per_part = int(np.prod(shape[1:])) * self._dsize(dtype)
        if top:
            off = (self.top_off - per_part) // align * align
            assert off >= self.sb_off, f"SBUF overflow (top) allocating {name}"
            self.n_names += 1
            t = self.nc.alloc_sbuf_tensor_at(f"{name}_{self.n_names}", list(shape), dtype, offset=off)
            self.top_off = off
            return Buf(name, t)
        off = (self.sb_off + align - 1) // align * align
        self.n_names += 1
        t = self.nc.alloc_sbuf_tensor_at(f"{name}_{self.n_names}", list(shape), dtype, offset=off)
        self.sb_off = off + per_part
        assert self.sb_off <= self.top_off, f"SBUF overflow allocating {name}: {self.sb_off} > {self.top_off}"
        self.sb_hi = max(self.sb_hi, self.sb_off)
        return Buf(name, t)

    def mark(self):
        return self.sb_off

    def free_top(self):
        self.top_off = self.sb_top

    def release(self, mark):
        self.sb_off = mark
        self.barrier()

    def psum(self, name, shape, dtype=F32):
        self.n_names += 1
        t = self.nc.alloc_psum_tensor(f"{name}_{self.n_names}", list(shape), dtype)
        return Buf(name, t)

    def dram(self, name, shape, dtype, kind="Internal"):
        t = self.nc.dram_tensor(name, list(shape), dtype, kind=kind)
        return Buf(name, t)

    def view(self, name="v"):
        return Buf(name, None)

    def _add(self, o, reads, writes):
        deps = {}
        for b in reads:
            if b.last_w is not None:
                deps[id(b.last_w)] = b.last_w
        for b in writes:
            if b.last_w is not None:
                deps[id(b.last_w)] = b.last_w
            for r in b.readers:
                deps[id(r)] = r
        for f in self.fence:
            deps[id(f)] = f
        deps.pop(id(o), None)
        o.deps = list(deps.values())
        for b in writes:
            b.last_w = o
            b.readers = []
        for b in reads:
            b.readers.append(o)
        o.gidx = len(self.all_ops)
        self.all_ops.append(o)
        self.ops[o.eng].append(o)
        return o

    def op(self, eng, fn, reads=(), writes=()):
        return self._add(Op(eng, fn), reads, writes)

    def dma(self, eng, out_ap, in_ap, reads=(), writes=(), semkey=None, **kw):
        if semkey is None:
            semkey = (writes[0].name if writes else reads[0].name) + "#" + str(id(writes[0] if writes else reads[0]))
        o = Op(eng, lambda e: e.dma_start(out=out_ap, in_=in_ap, **kw), is_dma=True, ndma=1, semkey=semkey)
        prev = self.last_dma.get(semkey)
        self._add(o, reads, writes)
        if prev is not None and all(d is not prev for d in o.deps):
            o.deps.append(prev)
        self.last_dma[semkey] = o
        return o

    def barrier(self):
        f = []
        for e in self.ENGS:
            for o in reversed(self.ops[e]):
                if not o.is_dma:
                    f.append(o)
                    break
        f.extend(self.last_dma.values())
        self.fence = f

    def finish(self):
        nc = self.nc
        for o in self.all_ops:
            for d in o.deps:
                if d.is_dma or not (d.eng == "pe" and o.eng == "pe" and not o.is_dma):
                    d.signal = True
        for o in self.last_dma.values():
            o.signal = True
        eng_sems = {}
        cnt = {}
        dma_sems = {}
        dma_cnt = {}
        final_dma = {}
        for o in self.all_ops:
            if o.is_dma:
                o.signal = True
                if o.semkey not in dma_sems:
                    dma_sems[o.semkey] = nc.alloc_semaphore(f"d{len(dma_sems)}")
                    dma_cnt[o.semkey] = 0
                dma_cnt[o.semkey] += 16 * o.ndma
                o.sem = dma_sems[o.semkey]
                o.val = dma_cnt[o.semkey]
                final_dma[o.semkey] = o
            elif o.signal:
                e = o.eng
                if e not in eng_sems or cnt[e] >= self.SEM_EPOCH:
                    eng_sems[e] = nc.alloc_semaphore(f"e_{e}_{len(eng_sems)}_{o.gidx}")
                    cnt[e] = 0
                cnt[e] += 1
                o.sem = eng_sems[e]
                o.val = cnt[e]
        self.n_sems = len(dma_sems)
        ops = self.ops
        final_list = list(final_dma.values())

        def emit(engname, eng):
            known = {}
            for o in ops[engname]:
                for d in o.deps:
                    if not d.signal:
                        continue
                    if (not d.is_dma) and d.eng == "pe" and engname == "pe" and not o.is_dma:
                        continue
                    k = id(d.sem)
                    if known.get(k, 0) >= d.val:
                        continue
                    eng.wait_ge(d.sem, d.val)
                    known[k] = d.val
                ins = o.fn(eng)
                if o.signal:
                    if o.is_dma:
                        ins.then_inc(o.sem, 16)
                    else:
                        ins.then_inc(o.sem, 1)
            if engname == "sp":
                for o in final_list:
                    k = id(o.sem)
                    if known.get(k, 0) >= o.val:
                        continue
                    eng.wait_ge(o.sem, o.val)
                    known[k] = o.val

        with nc.Block() as block:
            block.tensor(lambda e: emit("pe", e))
            block.scalar(lambda e: emit("act", e))
            block.vector(lambda e: emit("dve", e))
            block.gpsimd(lambda e: emit("pool", e))
            block.sync(lambda e: emit("sp", e))


D = 1024
S = 4096
NL = 2
EPS = 1e-6
TT = 512
NT = S // TT
KC = D // 128
IN_SIZES = (256, 128, 32, 512, 256, 256, 512, 4, 4, 512, 512, 512, 512, 512, 512, 1024, 512)
IN_OFF = [0]
for _s in IN_SIZES:
    IN_OFF.append(IN_OFF[-1] + _s)
(O_CQ, O_CKV, O_KR, O_GA, O_MQ, O_MK, O_MV, O_MI, O_MF, O_MO, O_GB,
 O_CX, O_CB, O_CC, O_GC, O_DUV, O_GD) = IN_OFF[:17]
IN_DIM = IN_OFF[-1]
TWO_PI = 6.283185307179586


class Ctx:
    pass


def _bc(ap, shape):
    return ap.to_broadcast(list(shape))


ALL_STAGES = ("attn", "mlstm", "conv", "sgu")


def build_program(n_layers, dbg=False, stages=ALL_STAGES):
    nc = bass.Bass("TRN2", target_bir_lowering=False)
    P = Planner(nc)
    c = Ctx()
    c.nc, c.P, c.dbg = nc, P, dbg

    def din(name, shape, dtype=F32):
        return nc.dram_tensor(name, list(shape), dtype, kind="ExternalInput").ap()

    c.x = din("x", [S, D])
    c.pos = din("pos", [1, S], I32)
    c.w_in = din("w_in", [n_layers, D, IN_DIM])
    c.w_gate = din("w_gate", [n_layers, D, 4 * D])
    c.w_uq = din("w_uq", [n_layers, 256, 768])
    c.w_uq_sw = din("w_uq_sw", [n_layers, 256, 768])
    c.w_ukv = din("w_ukv", [n_layers, 128, 1024])
    c.w_kr = din("w_kr", [n_layers, D, 2, 96])
    c.w_p = din("w_p", [n_layers, 4, 512, D])
    c.w_out = din("w_out", [n_layers, D, D])
    c.w_sT = din("w_sT", [n_layers, 4, 128, 128])
    c.vecs = din("vecs", [n_layers, 128, NVEC])
    c.rows = din("rows", [n_layers, NROW, 1024])
    c.consts = din("consts", [NCONST, 128, 128])
    c.inv_freq = din("inv_freq", [128, 4])
    c.out = nc.dram_tensor("out", [S, D], F32, kind="ExternalOutput").ap()
    c.hT = P.dram("hT_s", [128, KC, S], BF16)
    c.yT = [P.dram(f"yT_s{b}", [128, 4, S], BF16, kind=("ExternalOutput" if dbg else "Internal")) for b in range(4)]
    c.xmid = P.dram("xmid_s", [S, D], F32)
    c.xmid_views = [P.view(f"xmv{i}") for i in range(S // 128)]
    c.rope_tab = P.dram("rope_s", [2, 32, S], F32)
    for b_ in [c.hT, c.xmid, c.rope_tab] + c.yT:
        b_.t = b_.t.ap()
    c.ps = [P.psum(f"ps{i}", [128, 512], F32) for i in range(7)]
    c.pst = P.psum("pst", [128, 1024], BF16)
    c.ident = P.sbuf("ident", [128, 128], BF16)
    P.dma("pool", c.ident.t[:], c.consts[0], writes=[c.ident], semkey="const")
    c.cf = P.sbuf("cf", [128, NCONST, 128], F32)
    P.dma("sp", c.cf.t[:], c.consts.rearrange("n p f -> p n f"), writes=[c.cf], semkey="const2")
    c.cb = P.sbuf("cb", [128, NCONST, 128], BF16)
    P.dma("pool", c.cb.t[:], c.consts.rearrange("n p f -> p n f"), writes=[c.cb], semkey="const")

    stage_setup(c)
    for l in range(n_layers):
        c.l = l
        c.x_in = c.x if l == 0 else c.xmid.t
        c.x_in_buf = None if l == 0 else c.xmid
        last = (l == n_layers - 1)
        c.x_out = c.out if last else c.xmid.t
        c.x_out_buf = None if last else c.xmid
        m0 = P.mark()
        c.vec = P.sbuf("vec", [128, NVEC], F32)
        P.dma("sp", c.vec.t[:], c.vecs[l], writes=[c.vec], semkey="vec")
        if "attn" in stages:
            c.Wattn = w_attn(c)
        stage_h(c)
        if "attn" in stages:
            P.release(c.attn_stg_mark)
        c.yT_src = [None] * 4
        for bi, (nm, fn, vn) in enumerate(STAGE_FNS):
            if nm in stages:
                c.after_w = None
                if nm == "mlstm" and "conv" in stages:
                    c.after_w = lambda: setattr(c, "Wconv", w_conv(c))
                if nm == "conv" and "mlstm" not in stages:
                    c.Wconv = w_conv(c)
                if nm == "sgu":
                    c.after_w = lambda: setattr(c, "Wfinal", w_final(c))
                fn(c)
                if nm in ("attn", "conv"):
                    P.free_top()
                c.yT_src[bi] = c.yT[bi].t
            else:
                c.yT_src[bi] = din(f"yTin{bi}_{l}", [128, 4, S], BF16)
                setattr(c, vn, [P.view(f"{vn}{i}") for i in range(NT)])
        if "sgu" not in stages:
            c.Wfinal = w_final(c)
        stage_final(c)
        P.free_top()
        P.release(m0)
    P.finish()
    return nc, P


V_GPRE = 0
V_WCONV = 8
V_BCONV = 20
V_GSV = 24
V_BGATE = 28
V_GCQ = 60
V_GCKV = 62
NVEC = 64
R_GPOST = 0
R_GMH = 1
R_BS = 2
R_FB = 3
NROW = 4
C_IDENT = 0
C_TRI = 1
C_BLK = 2
C_TRIL128 = 3
C_SEL0 = 4
C_SEL1 = 5
C_ONES = 6
C_SELDEN = 7
NCONST = 8


def K(buf):
    return buf.name


def stage_setup(c):
    P = c.P
    m0 = P.mark()
    invf = P.sbuf("invf", [128, 4], F32)
    P.dma("sp", invf.t[:], c.inv_freq, writes=[invf], semkey="invf")
    posi = P.sbuf("posi", [32, S], I32)
    P.dma("sp", posi.t[:], c.pos.to_broadcast([32, S]), writes=[posi], semkey="posi")
    ang = P.sbuf("ang", [32, S], F32)
    P.op("dve", lambda e: e.tensor_copy(ang.t[:], posi.t[:]), reads=[posi], writes=[ang])
    P.op("dve", lambda e: e.tensor_scalar(ang.t[:], ang.t[:], invf.t[0:32, 0:1], None, ALU.mult), reads=[ang, invf], writes=[ang])
    ki = P.sbuf("angk", [32, S], I32)
    tmp = P.sbuf("angt", [32, S], F32)
    tmp2 = P.sbuf("angt2", [32, S], F32)
    tab = P.sbuf("tab", [32, S], F32)

    def frac_sin(off, scale_arg, slot, key):
        P.op("dve", lambda e: e.tensor_scalar(tmp.t[:], ang.t[:], float(1.0 / TWO_PI), float(off), ALU.mult, ALU.add), reads=[ang], writes=[tmp])
        P.op("dve", lambda e: e.tensor_copy(ki.t[:], tmp.t[:]), reads=[tmp], writes=[ki])
        P.op("dve", lambda e: e.tensor_copy(tmp2.t[:], ki.t[:]), reads=[ki], writes=[tmp2])
        P.op("dve", lambda e: e.tensor_tensor(tmp.t[:], tmp.t[:], tmp2.t[:], ALU.subtract), reads=[tmp, tmp2], writes=[tmp])
        P.op("dve", lambda e: e.tensor_single_scalar(tmp2.t[:], tmp.t[:], 0.5, ALU.is_gt), reads=[tmp], writes=[tmp2])
        P.op("dve", lambda e: e.tensor_tensor(tmp.t[:], tmp.t[:], tmp2.t[:], ALU.subtract), reads=[tmp, tmp2], writes=[tmp])
        P.op("act", lambda e: e.activation(tab.t[:], tmp.t[:], AF.Sin, scale=scale_arg), reads=[tmp, invf], writes=[tab])
        P.dma("sp", c.rope_tab.t[slot], tab.t[:], reads=[tab], writes=[c.rope_tab], semkey=key)

    frac_sin(0.25, TWO_PI, 0, "tab")
    frac_sin(0.0, invf.t[0:32, 1:2], 1, "tab")
    P.release(m0)


def rstd_from_ss(P, out_ap, ss_ap, n, reads, writes, eng="dve"):
    P.op("act", lambda e: e.activation(out_ap, ss_ap, AF.Sqrt, bias=EPS, scale=1.0 / n), reads=reads, writes=writes)
    P.op(eng, lambda e: e.reciprocal(out_ap, out_ap), reads=writes, writes=writes)


def stage_h(c):
    P = c.P
    m0 = P.mark()
    NB = 6
    DEPTH = 4
    xt = [P.sbuf(f"xt{i}", [128, D], F32) for i in range(NB)]
    junk = P.sbuf("hjunk", [128, D], F32)
    hb = [P.sbuf(f"hb{i}", [128, D], BF16) for i in range(2)]
    ht = [P.sbuf(f"ht{i}", [128, KC, 128], BF16) for i in range(2)]
    ss = [P.sbuf(f"hss{i}", [128, 1], F32) for i in range(2)]
    rs = [P.sbuf(f"hrs{i}", [128, 1], F32) for i in range(2)]
    c.hT_views = [P.view(f"hTv{i}") for i in range(S // 128)]
    gpre = c.vec.t[:, V_GPRE:V_GPRE + KC]
    pst = c.pst
    nt = S // 128

    def load(i):
        x_ = xt[i % NB]
        rd = [c.xmid_views[i]] if c.x_in_buf is not None else []
        P.dma("sp", x_.t[:], c.x_in[i * 128:(i + 1) * 128, :], reads=rd, writes=[x_], semkey=K(x_))

    def stats(i):
        x_, s_, r_ = xt[i % NB], ss[i % 2], rs[i % 2]
        P.op("dve", lambda e, s_=s_: e.memset(s_.t[:], 0.0), writes=[s_])
        P.op("act", lambda e, x_=x_, s_=s_: e.activation(junk.t[:], x_.t[:], AF.Square, accum_out=s_.t[:]), reads=[x_, s_], writes=[junk, s_])
        rstd_from_ss(P, r_.t[:], s_.t[:], D, [s_], [r_])

    def apply(i):
        x_, r_, hb_, ht_ = xt[i % NB], rs[i % 2], hb[i % 2], ht[i % 2]
        P.op("act", lambda e, x_=x_, r_=r_, hb_=hb_: e.activation(hb_.t[:], x_.t[:], AF.Copy, scale=r_.t[:]), reads=[x_, r_], writes=[hb_])
        for kc in range(KC):
            P.op("pe", lambda e, kc=kc, hb_=hb_: e.transpose(pst.t[:, kc * 128:(kc + 1) * 128], hb_.t[:, kc * 128:(kc + 1) * 128], c.ident.t[:]),
                 reads=[hb_, c.ident], writes=[pst])
        P.op("dve", lambda e, ht_=ht_: e.tensor_tensor(ht_.t[:], pst.t[:].rearrange("p (k t) -> p k t", k=KC),
                                                      gpre.unsqueeze(2).to_broadcast([128, KC, 128]), ALU.mult),
             reads=[pst, c.vec], writes=[ht_])
        P.dma("sp", c.hT.t[:, :, i * 128:(i + 1) * 128], ht_.t[:], reads=[ht_], writes=[c.hT_views[i]], semkey=K(ht_))

    for i in range(DEPTH):
        load(i)
    stats(0)
    for i in range(nt):
        if i + DEPTH < nt:
            load(i + DEPTH)
        if i + 1 < nt:
            stats(i + 1)
        apply(i)
    P.release(m0)


def load_w(c, dst, src_ap, key):
    c.P.dma("pool", dst.t[:] if not isinstance(dst, tuple) else dst[1], src_ap, writes=[dst if not isinstance(dst, tuple) else dst[0]], semkey=key)


def load_hT(c, buf, T):
    P = c.P
    rd = c.hT_views[T * 4:(T + 1) * 4]
    P.dma("sp", buf.t[:], c.hT.t[:, :, T * TT:(T + 1) * TT], reads=rd, writes=[buf], semkey=K(buf))


def proj_T(c, ps, w_ap_fn, hT_buf, wbuf, n_k=KC, M=128, ncols=TT, col0=0, extra_reads=()):
    P = c.P
    for kc in range(n_k):
        P.op("pe", lambda e, kc=kc: e.matmul(ps.t[0:M, 0:ncols], w_ap_fn(kc), hT_buf.t[:, kc, col0:col0 + ncols],
                                             start=(kc == 0), stop=(kc == n_k - 1)),
             reads=[wbuf, hT_buf] + list(extra_reads), writes=[ps])


def w_conv(c):
    P, l = c.P, c.l
    w = P.sbuf("wconv", [128, KC, 2048], BF16, top=True)
    for kc in range(KC):
        P.dma("pool", w.t[:, kc, :], c.w_in[l, kc * 128:(kc + 1) * 128, O_CX:O_CX + 2048], writes=[w], semkey="wconv")
    return w


def stage_conv(c):
    P = c.P
    l = c.l
    m0 = P.mark()
    w = c.Wconv
    hT = [P.sbuf(f"chT{i}", [128, KC, TT], BF16) for i in range(2)]
    u = P.sbuf("cu", [128, 4, TT + 2], F32)
    cxs = P.sbuf("ccx", [128, TT], F32)
    y = P.sbuf("cy", [128, TT], F32)
    sg = P.sbuf("csg", [128, TT], F32)
    yo = [P.sbuf(f"cyo{i}", [128, 4, TT], BF16) for i in range(2)]
    P.op("dve", lambda e: e.memset(u.t[:], 0.0), writes=[u])
    ps = c.ps
    c.yc_views = [P.view(f"ycv{i}") for i in range(NT)]
    load_hT(c, hT[0], 0)
    for T in range(NT):
        h_ = hT[T % 2]
        if T + 1 < NT:
            load_hT(c, hT[(T + 1) % 2], T + 1)
        yo_ = yo[T % 2]
        for ch in range(4):
            par = (T * 4 + ch) % 2
            pcx, pcc, pgc, pcb = ps[0 + par], ps[2 + par], ps[4 + par], ps[6]
            for j, pp in ((0, pcx), (2, pcc), (3, pgc), (1, pcb)):
                proj_T(c, pp, lambda kc, j=j, ch=ch: w.t[:, kc, j * 512 + ch * 128: j * 512 + (ch + 1) * 128], h_, w)
            wc = lambda k, ch=ch: c.vec.t[:, V_WCONV + ch * 3 + k: V_WCONV + ch * 3 + k + 1]
            P.op("act", lambda e, pcx=pcx: e.activation(cxs.t[:], pcx.t[:], AF.Copy), reads=[pcx], writes=[cxs])
            P.op("act", lambda e, pgc=pgc: e.activation(sg.t[:], pgc.t[:], AF.Silu), reads=[pgc], writes=[sg])
            P.op("dve", lambda e, ch=ch, pcc=pcc: e.tensor_tensor(u.t[:, ch, 2:TT + 2], pcc.t[:], cxs.t[:], ALU.mult), reads=[pcc, cxs], writes=[u])
            P.op("dve", lambda e, ch=ch, wc=wc: e.tensor_scalar(y.t[:], u.t[:, ch, 2:TT + 2], wc(2), None, ALU.mult), reads=[u, c.vec], writes=[y])
            P.op("dve", lambda e, ch=ch, wc=wc: e.scalar_tensor_tensor(y.t[:], u.t[:, ch, 1:TT + 1], wc(1), y.t[:], ALU.mult, ALU.add), reads=[u, y, c.vec], writes=[y])
            P.op("dve", lambda e, ch=ch, wc=wc: e.scalar_tensor_tensor(y.t[:], u.t[:, ch, 0:TT], wc(0), y.t[:], ALU.mult, ALU.add), reads=[u, y, c.vec], writes=[y])
            P.op("dve", lambda e, ch=ch, pcb=pcb: e.scalar_tensor_tensor(y.t[:], y.t[:], c.vec.t[:, V_BCONV + ch:V_BCONV + ch + 1], pcb.t[:], ALU.add, ALU.mult),
                 reads=[y, pcb, c.vec], writes=[y])
            P.op("dve", lambda e, ch=ch, yo_=yo_: e.tensor_tensor(yo_.t[:, ch, :], y.t[:], sg.t[:], ALU.mult), reads=[y, sg], writes=[yo_])
            P.op("dve", lambda e, ch=ch: e.tensor_copy(u.t[:, ch, 0:2], u.t[:, ch, TT:TT + 2]), reads=[u], writes=[u])
        P.dma("pool", c.yT[2].t[:, :, T * TT:(T + 1) * TT], yo_.t[:], reads=[yo_], writes=[c.yc_views[T]], semkey=K(yo_))
    P.release(m0)


def w_final(c):
    P, l = c.P, c.l
    wg = P.sbuf("wg", [128, KC, 4 * D], BF16, top=True)
    for kc in range(KC):
        P.dma("pool", wg.t[:, kc, :], c.w_gate[l, kc * 128:(kc + 1) * 128, :], writes=[wg], semkey="wg")
    wp = P.sbuf("wp", [128, 4, 4, D], BF16, top=True)
    for b in range(4):
        P.dma("pool", wp.t[:, b, :, :], c.w_p[l, b].rearrange("(k p) n -> p k n", p=128), writes=[wp], semkey="wp")
    wo = P.sbuf("wo", [128, KC, D], BF16, top=True)
    P.dma("pool", wo.t[:], c.w_out[l].rearrange("(k p) n -> p k n", p=128), writes=[wo], semkey="wo")
    gpost = P.sbuf("gpost", [128, D], F32, top=True)
    P.dma("sp", gpost.t[:], c.rows[l, R_GPOST:R_GPOST + 1, :].to_broadcast([128, D]), writes=[gpost], semkey="gpost")
    return wg, wp, wo, gpost


def stage_final(c):
    P = c.P
    l = c.l
    m0 = P.mark()
    wg, wp, wo, gpost = c.Wfinal
    hbuf = [P.sbuf(f"fhT{i}", [128, KC, TT], BF16) for i in range(2)]
    ytb = [P.sbuf(f"fyt{b}", [128, 4, TT], BF16) for b in range(4)]
    sig = [P.sbuf(f"fsig{i}", [128, TT], F32) for i in range(2)]
    acc = P.sbuf("facc", [128, TT], F32)
    tmp = [P.sbuf(f"ftmp{i}", [128, TT], F32) for i in range(2)]
    mTb = [P.sbuf(f"fmT{i}", [128, KC, TT], BF16) for i in range(2)]
    xt = [P.sbuf(f"fxt{i}", [128, D], F32) for i in range(2)]
    ot = [P.sbuf(f"fot{i}", [128, D], F32) for i in range(2)]
    junk = P.sbuf("fjunk", [128, TT], F32)
    ss2 = [P.sbuf(f"fss{i}", [128, 2], F32) for i in range(2)]
    rs = [P.sbuf(f"frs{i}", [128, 1], F32) for i in range(2)]
    ps = c.ps
    yviews = [c.ya_views, c.yb_views, c.yc_views, c.yd_views]

    def gate_block(T, cch, h_, mT):
        for b in range(4):
            pg, pp, sg_ = ps[b % 2], ps[2 + b % 2], sig[b % 2]
            proj_T(c, pg, lambda kc, b=b, cch=cch: wg.t[:, kc, b * D + cch * 128: b * D + (cch + 1) * 128], h_, wg)
            bcol = c.vec.t[:, V_BGATE + b * 8 + cch: V_BGATE + b * 8 + cch + 1]
            P.op("act", lambda e, pg=pg, sg_=sg_, bcol=bcol: e.activation(sg_.t[:], pg.t[:], AF.Sigmoid, bias=bcol), reads=[pg, c.vec], writes=[sg_])
            for kc in range(4):
                P.op("pe", lambda e, kc=kc, b=b, cch=cch, pp=pp: e.matmul(pp.t[:], wp.t[:, b, kc, cch * 128:(cch + 1) * 128], ytb[b].t[:, kc, :],
                                                                   start=(kc == 0), stop=(kc == 3)), reads=[wp, ytb[b]], writes=[pp])
            if b == 0:
                P.op("dve", lambda e, pp=pp, sg_=sg_: e.tensor_tensor(acc.t[:], pp.t[:], sg_.t[:], ALU.mult), reads=[pp, sg_], writes=[acc])
            else:
                t_ = tmp[b % 2]
                P.op("dve", lambda e, pp=pp, sg_=sg_, t_=t_: e.tensor_tensor(t_.t[:], pp.t[:], sg_.t[:], ALU.mult), reads=[pp, sg_], writes=[t_])
                if b < 3:
                    P.op("pool", lambda e, t_=t_: e.tensor_tensor(acc.t[:], acc.t[:], t_.t[:], ALU.add), reads=[acc, t_], writes=[acc])
                else:
                    P.op("pool", lambda e, t_=t_, cch=cch, mT=mT: e.tensor_tensor(mT.t[:, cch, :], acc.t[:], t_.t[:], ALU.add), reads=[acc, t_], writes=[mT])

    def out_tile(T, t4, mT):
        i = T * 4 + t4
        x_, o_, s_, r_ = xt[i % 2], ot[i % 2], ss2[i % 2], rs[i % 2]
        rd = [c.xmid_views[i]] if c.x_in_buf is not None else []
        P.dma("sp", x_.t[:], c.x_in[i * 128:(i + 1) * 128, :], reads=rd, writes=[x_], semkey=K(x_))
        P.op("dve", lambda e, s_=s_: e.memset(s_.t[:], 0.0), writes=[s_])
        for half in range(2):
            po = ps[4 + half]
            for kc in range(KC):
                P.op("pe", lambda e, kc=kc, half=half, po=po, t4=t4, mT=mT: e.matmul(po.t[:], mT.t[:, kc, t4 * 128:(t4 + 1) * 128], wo.t[:, kc, half * 512:(half + 1) * 512],
                                                                       start=(kc == 0), stop=(kc == KC - 1)), reads=[mT, wo], writes=[po])
            P.op("act", lambda e, po=po, s_=s_, half=half: e.activation(junk.t[:], po.t[:], AF.Square, accum_out=s_.t[:, half:half + 1]),
                 reads=[po, s_], writes=[junk, s_])
        P.op("dve", lambda e, s_=s_: e.tensor_tensor(s_.t[:, 0:1], s_.t[:, 0:1], s_.t[:, 1:2], ALU.add), reads=[s_], writes=[s_])
        rstd_from_ss(P, r_.t[:], s_.t[:, 0:1], D, [s_], [r_])
        for half in range(2):
            po = ps[4 + half]
            sl = slice(half * 512, (half + 1) * 512)
            P.op("dve", lambda e, po=po, sl=sl, r_=r_, o_=o_: e.scalar_tensor_tensor(o_.t[:, sl], po.t[:], r_.t[:], gpost.t[:, sl], ALU.mult, ALU.mult),
                 reads=[po, r_, gpost], writes=[o_])
        P.op("pool", lambda e, o_=o_, x_=x_: e.tensor_tensor(o_.t[:], o_.t[:], x_.t[:], ALU.add), reads=[o_, x_], writes=[o_])
        wr = [c.xmid_views[i]] if c.x_out_buf is not None else []
        P.dma("pool", c.x_out[i * 128:(i + 1) * 128, :], o_.t[:], reads=[o_], writes=wr, semkey=K(o_))

    load_hT(c, hbuf[0], 0)
    for T in range(NT + 1):
        if T < NT:
            h_ = hbuf[T % 2]
            for b in range(4):
                P.dma("sp", ytb[b].t[:], c.yT_src[b][:, :, T * TT:(T + 1) * TT], reads=[yviews[b][T]], writes=[ytb[b]], semkey=K(ytb[b]))
            if T + 1 < NT:
                load_hT(c, hbuf[(T + 1) % 2], T + 1)
        for cch in range(KC):
            if T < NT:
                gate_block(T, cch, h_, mTb[T % 2])
            if T >= 1 and cch % 2 == 1:
                out_tile(T - 1, cch // 2, mTb[(T - 1) % 2])
    P.release(m0)


def stage_sgu(c):
    P = c.P
    l = c.l
    m0 = P.mark()
    w = P.sbuf("wsgu", [128, KC, 1536], BF16)
    for kc in range(KC):
        P.dma("pool", w.t[:, kc, 0:1024], c.w_in[l, kc * 128:(kc + 1) * 128, O_DUV:O_DUV + 1024], writes=[w], semkey="wsgu")
        P.dma("pool", w.t[:, kc, 1024:1536], c.w_in[l, kc * 128:(kc + 1) * 128, O_GD:O_GD + 512], writes=[w], semkey="wsgu")
    wsf = P.sbuf("wsf", [128, 4, 128], F32)
    P.dma("sp", wsf.t[:], c.w_sT[l].rearrange("g s t -> s g t"), writes=[wsf], semkey="wsf")
    wsm = P.sbuf("wsm", [128, 4, 128], BF16)
    P.op("dve", lambda e: e.tensor_tensor(wsm.t[:], wsf.t[:], c.cf.t[:, C_TRIL128:C_TRIL128 + 1, :].to_broadcast([128, 4, 128]), ALU.mult),
         reads=[wsf, c.cf], writes=[wsm])
    bsb = P.sbuf("bsb", [128, 512], F32)
    P.dma("sp", bsb.t[:], c.rows[l, R_BS:R_BS + 1, 0:512].to_broadcast([128, 512]), writes=[bsb], semkey="bsb")
    if c.after_w is not None:
        c.after_w()
    hT = [P.sbuf(f"dhT{i}", [128, KC, TT], BF16) for i in range(2)]
    ug = P.sbuf("dug", [128, 4, TT], F32)
    sgd = P.sbuf("dsgd", [128, TT], F32)
    vg = [P.sbuf(f"dvg{i}", [128, 512], F32) for i in range(2)]
    junk = P.sbuf("djunk", [128, 512], F32)
    st = [P.sbuf(f"dst{i}", [128, 4], F32) for i in range(2)]
    vn = [P.sbuf(f"dvn{i}", [128, 512], BF16) for i in range(8)]
    mx = P.sbuf("dmx", [128, 4, 128], F32)
    yo = [P.sbuf(f"dyo{i}", [128, 4, TT], BF16) for i in range(2)]
    ps = c.ps
    c.yd_views = [P.view(f"ydv{i}") for i in range(NT)]
    load_hT(c, hT[0], 0)

    def vpart(T):
        h_ = hT[T % 2]
        for t4 in range(4):
            pv, vg_, st_, vn_ = ps[2 + t4 % 2], vg[t4 % 2], st[t4 % 2], vn[(T % 2) * 4 + t4]
            for kc in range(KC):
                P.op("pe", lambda e, kc=kc, t4=t4, pv=pv, h_=h_: e.matmul(pv.t[:], h_.t[:, kc, t4 * 128:(t4 + 1) * 128], w.t[:, kc, 512:1024],
                                                            start=(kc == 0), stop=(kc == KC - 1)), reads=[h_, w], writes=[pv])
            P.op("dve", lambda e, st_=st_: e.memset(st_.t[:], 0.0), writes=[st_])
            P.op("act", lambda e, pv=pv, vg_=vg_, st_=st_: e.activation(vg_.t[:], pv.t[:], AF.Gelu_apprx_tanh, accum_out=st_.t[:, 0:1]),
                 reads=[pv, st_], writes=[vg_, st_])
            P.op("act", lambda e, vg_=vg_, st_=st_: e.activation(junk.t[:], vg_.t[:], AF.Square, accum_out=st_.t[:, 1:2]),
                 reads=[vg_, st_], writes=[junk, st_])
            P.op("dve", lambda e, st_=st_: e.tensor_scalar(st_.t[:, 0:2], st_.t[:, 0:2], 1.0 / 512, None, ALU.mult), reads=[st_], writes=[st_])
            P.op("dve", lambda e, st_=st_: e.tensor_tensor(st_.t[:, 2:3], st_.t[:, 0:1], st_.t[:, 0:1], ALU.mult), reads=[st_], writes=[st_])
            P.op("dve", lambda e, st_=st_: e.tensor_tensor(st_.t[:, 3:4], st_.t[:, 1:2], st_.t[:, 2:3], ALU.subtract), reads=[st_], writes=[st_])
            P.op("act", lambda e, st_=st_: e.activation(st_.t[:, 3:4], st_.t[:, 3:4], AF.Sqrt, bias=EPS, scale=1.0), reads=[st_], writes=[st_])
            P.op("dve", lambda e, st_=st_: e.reciprocal(st_.t[:, 3:4], st_.t[:, 3:4]), reads=[st_], writes=[st_])
            P.op("dve", lambda e, st_=st_, vg_=vg_, vn_=vn_: e.tensor_scalar(vn_.t[:], vg_.t[:], st_.t[:, 0:1], st_.t[:, 3:4], ALU.subtract, ALU.mult),
                 reads=[vg_, st_], writes=[vn_])

    def upart(T, yo_):
        h_ = hT[T % 2]
        for ch in range(4):
            pu, pg = ps[0], ps[1]
            if ch % 2 == 1:
                pu, pg = ps[6], ps[4]
            proj_T(c, pu, lambda kc, ch=ch: w.t[:, kc, ch * 128:(ch + 1) * 128], h_, w)
            proj_T(c, pg, lambda kc, ch=ch: w.t[:, kc, 1024 + ch * 128:1024 + (ch + 1) * 128], h_, w)
            P.op("act", lambda e, ch=ch, pu=pu: e.activation(ug.t[:, ch, :], pu.t[:], AF.Gelu_apprx_tanh), reads=[pu], writes=[ug])
            P.op("act", lambda e, pg=pg: e.activation(sgd.t[:], pg.t[:], AF.Silu), reads=[pg], writes=[sgd])
            P.op("dve", lambda e, ch=ch: e.tensor_tensor(ug.t[:, ch, :], ug.t[:, ch, :], sgd.t[:], ALU.mult), reads=[ug, sgd], writes=[ug])

    def mpart(T, yo_):
        for g in range(4):
            pm = ps[4 + g % 2]
            for t4 in range(4):
                P.op("pe", lambda e, g=g, t4=t4, pm=pm, T=T: e.matmul(pm.t[:, t4 * 128:(t4 + 1) * 128], vn[(T % 2) * 4 + t4].t[:, g * 128:(g + 1) * 128], wsm.t[:, g, :],
                                                          start=True, stop=True), reads=[vn[(T % 2) * 4 + t4], wsm], writes=[pm])
            P.op("dve", lambda e, g=g, pm=pm: e.scalar_tensor_tensor(mx.t[:], pm.t[:].rearrange("p (a t) -> p a t", a=4),
                                                                   c.vec.t[:, V_GSV + g:V_GSV + g + 1],
                                                                   bsb.t[:, g * 128:(g + 1) * 128].unsqueeze(1).to_broadcast([128, 4, 128]), ALU.mult, ALU.add),
                 reads=[pm, c.vec, bsb], writes=[mx])
            P.op("dve", lambda e, g=g, yo_=yo_: e.tensor_tensor(yo_.t[:, g, :], mx.t[:].rearrange("p a t -> p (a t)"), ug.t[:, g, :], ALU.mult),
                 reads=[mx, ug], writes=[yo_])
        P.dma("pool", c.yT[3].t[:, :, T * TT:(T + 1) * TT], yo_.t[:], reads=[yo_], writes=[c.yd_views[T]], semkey=K(yo_))

    vpart(0)
    for T in range(NT):
        if T + 1 < NT:
            load_hT(c, hT[(T + 1) % 2], T + 1)
        yo_ = yo[T % 2]
        upart(T, yo_)
        if T + 1 < NT:
            vpart(T + 1)
        mpart(T, yo_)
    P.release(m0)


def w_attn(c):
    P, l = c.P, c.l
    c.attn_stg_mark = P.mark()
    wA = P.sbuf("wA", [128, KC, 896], BF16, top=True)
    for kc in range(KC):
        P.dma("pool", wA.t[:, kc, 0:384], c.w_in[l, kc * 128:(kc + 1) * 128, O_CQ:O_CQ + 384], writes=[wA], semkey="wA")
        P.dma("pool", wA.t[:, kc, 384:896], c.w_in[l, kc * 128:(kc + 1) * 128, O_GA:O_GA + 512], writes=[wA], semkey="wA")
    wkr = P.sbuf("wkr", [128, KC, 2, 96], BF16, top=True)
    P.dma("pool", wkr.t[:], c.w_kr[l].rearrange("(k p) a n -> p k a n", p=128), writes=[wkr], semkey="wkr")
    wuq = P.sbuf("wuq", [128, 2, 768], BF16, top=True)
    wuqs = P.sbuf("wuqs", [128, 2, 768], BF16, top=True)
    wk = P.sbuf("wk", [128, 8, 64], BF16, top=True)
    wv = P.sbuf("wv", [128, 8, 64], BF16, top=True)
    stg = P.sbuf("astg", [128, 2, 768], F32)
    stg2 = P.sbuf("astg2", [128, 8, 128], F32)
    for src, dst in ((c.w_uq, wuq), (c.w_uq_sw, wuqs)):
        P.dma("sp", stg.t[:], src[l].rearrange("(k p) n -> p k n", p=128), writes=[stg], semkey="astg")
        for j in range(2):
            P.op("dve", lambda e, j=j, dst=dst: e.tensor_scalar(dst.t[:, j, :], stg.t[:, j, :], c.vec.t[:, V_GCQ + j:V_GCQ + j + 1], None, ALU.mult),
                 reads=[stg, c.vec], writes=[dst])
    P.dma("sp", stg2.t[:], c.w_ukv[l].rearrange("k (h x) -> k h x", x=128), writes=[stg2], semkey="astg2")
    gk = c.vec.t[:, V_GCKV:V_GCKV + 1]
    P.op("dve", lambda e: e.tensor_scalar(wk.t[:], stg2.t[:, :, 0:64], gk, None, ALU.mult), reads=[stg2, c.vec], writes=[wk])
    P.op("dve", lambda e: e.tensor_scalar(wv.t[:], stg2.t[:, :, 64:128], gk, None, ALU.mult), reads=[stg2, c.vec], writes=[wv])
    return wA, wkr, wuq, wuqs, wk, wv


def stage_attn(c):
    P = c.P
    l = c.l
    m0 = P.mark()
    ps = c.ps
    cf = c.cf
    wA, wkr, wuq, wuqs, wk, wv = c.Wattn
    KT = P.sbuf("KT", [128, 8, S], BF16)
    VA = P.sbuf("VA", [128, S // 128, 8, 72], BF16)
    P.op("pool", lambda e: e.memset(VA.t[:, :, :, 64:65], 1.0), writes=[VA])
    h_ = P.sbuf("ahT", [128, KC, TT], BF16)
    cqb = P.sbuf("cqb", [128, 2, TT], BF16)
    ckvb = P.sbuf("ckvb", [128, TT], BF16)
    sqf = [P.sbuf(f"sqf{i}", [128, TT], F32) for i in range(2)]
    rq_b = P.sbuf("rq_b", [128, TT], F32)
    rkv_b = P.sbuf("rkv_b", [128, TT], F32)
    rkv_tok = P.sbuf("rkv_tok", [128, 4], F32)
    C2t = P.sbuf("C2t", [128, TT], F32)
    S2t = P.sbuf("S2t", [128, TT], F32)
    C2r = P.sbuf("C2r", [128, TT], F32)
    S2r = P.sbuf("S2r", [128, TT], F32)
    kr = P.sbuf("kr", [128, TT], BF16)
    t1 = P.sbuf("at1", [128, TT], F32)
    t2 = P.sbuf("at2", [128, TT], F32)
    t1b = P.sbuf("at1b", [128, TT], F32)
    t2b = P.sbuf("at2b", [128, TT], F32)
    nb = [0]
    QT = P.sbuf("QT", [128, 8, TT], BF16)
    PT = [P.sbuf(f"PT{i}", [128, TT], BF16) for i in range(3)]
    Osb = [P.sbuf(f"Osb{i}", [128, TT], F32) for i in range(2)]
    rden = P.sbuf("rden", [128, TT], F32)
    yst = [P.sbuf(f"yst{i}", [128, TT], F32) for i in range(2)]
    sga4 = P.sbuf("sga4", [128, 4, TT], BF16)
    yo = [P.sbuf(f"ayo{i}", [128, 4, TT], BF16) for i in range(2)]
    c.ya_views = [P.view(f"yav{i}") for i in range(NT)]
    scale = float(96 ** -0.5)
    ones_f = cf.t[:, C_ONES, :]
    R = slice(64, 96)
    nS = 0
    for T in range(NT):
        cols = slice(T * TT, (T + 1) * TT)
        if T == 0:
            load_hT(c, h_, 0)
        P.dma("sp", C2t.t[R, :], c.rope_tab.t[0][:, cols], reads=[c.rope_tab], writes=[C2t], semkey="C2t")
        P.dma("sp", S2t.t[R, :], c.rope_tab.t[1][:, cols], reads=[c.rope_tab], writes=[S2t], semkey="S2t")

        def nxt():
            b_ = ps[nb[0] % 7]
            nb[0] += 1
            return b_

        pcq = [nxt(), nxt()]
        for j in range(2):
            proj_T(c, pcq[j], lambda kc, j=j: wA.t[:, kc, j * 128:(j + 1) * 128], h_, wA)
            P.op("act", lambda e, j=j, pp=pcq[j]: e.activation(cqb.t[:, j, :], pp.t[:], AF.Copy), reads=[pcq[j]], writes=[cqb])
            P.op("act", lambda e, j=j, pp=pcq[j]: e.activation(sqf[j].t[:], pp.t[:], AF.Square), reads=[pcq[j]], writes=[sqf[j]])
        pkv = nxt()
        proj_T(c, pkv, lambda kc: wA.t[:, kc, 256:384], h_, wA)
        pss = nxt()
        for j in range(2):
            P.op("pe", lambda e, j=j, pss=pss: e.matmul(pss.t[:], ones_f, sqf[j].t[:], start=(j == 0), stop=(j == 1)), reads=[cf, sqf[j]], writes=[pss])
        rstd_from_ss(P, rq_b.t[:], pss.t[:], 256, [pss], [rq_b])
        P.op("act", lambda e, pkv=pkv: e.activation(ckvb.t[:], pkv.t[:], AF.Copy), reads=[pkv], writes=[ckvb])
        P.op("act", lambda e, pkv=pkv: e.activation(sqf[0].t[:], pkv.t[:], AF.Square), reads=[pkv], writes=[sqf[0]])
        pkr = [nxt(), nxt()]
        for a in range(2):
            proj_T(c, pkr[a], lambda kc, a=a: wkr.t[:, kc, a, :], h_, wkr, M=96)
        P.op("dve", lambda e, pp=pkr[0]: e.tensor_tensor(t1.t[R, :], pp.t[R, :], C2t.t[R, :], ALU.mult), reads=[pkr[0], C2t], writes=[t1])
        P.op("dve", lambda e, pp=pkr[1]: e.tensor_tensor(t2.t[R, :], pp.t[R, :], S2t.t[R, :], ALU.mult), reads=[pkr[1], S2t], writes=[t2])
        P.op("pool", lambda e: e.tensor_tensor(kr.t[R, :], t1.t[R, :], t2.t[R, :], ALU.add), reads=[t1, t2], writes=[kr])
        P.op("pool", lambda e, cols=cols: e.tensor_copy(KT.t[R, :, cols], kr.t[R, :].unsqueeze(1).to_broadcast([32, 8, TT])), reads=[kr], writes=[KT])
        for ch in range(4):
            pga = nxt()
            proj_T(c, pga, lambda kc, ch=ch: wA.t[:, kc, 384 + ch * 128:384 + (ch + 1) * 128], h_, wA)
            P.op("act", lambda e, ch=ch, pga=pga: e.activation(sga4.t[:, ch, :], pga.t[:], AF.Silu), reads=[pga], writes=[sga4])
        if T + 1 < NT:
            load_hT(c, h_, T + 1)
        pss2 = nxt()
        P.op("pe", lambda e, pss2=pss2: e.matmul(pss2.t[:], ones_f, sqf[0].t[:], start=True, stop=True), reads=[cf, sqf[0]], writes=[pss2])
        rstd_from_ss(P, rkv_b.t[:], pss2.t[:], 128, [pss2], [rkv_b])
        pst_ = nxt()
        for t4 in range(4):
            P.op("pe", lambda e, t4=t4, pst_=pst_: e.matmul(pst_.t[:, t4:t4 + 1], sqf[0].t[:, t4 * 128:(t4 + 1) * 128], cf.t[:, C_ONES, 0:1], start=True, stop=True),
                 reads=[cf, sqf[0]], writes=[pst_])
        rstd_from_ss(P, rkv_tok.t[:], pst_.t[:, 0:4], 128, [pst_], [rkv_tok])
        P.op("pool", lambda e: e.tensor_tensor(C2r.t[R, :], C2t.t[R, :], rq_b.t[R, :], ALU.mult), reads=[C2t, rq_b], writes=[C2r])
        P.op("pool", lambda e: e.tensor_tensor(S2r.t[R, :], S2t.t[R, :], rq_b.t[R, :], ALU.mult), reads=[S2t, rq_b], writes=[S2r])
        for h in range(8):
            pk = nxt()
            P.op("pe", lambda e, h=h, pk=pk: e.matmul(pk.t[0:64, :], wk.t[:, h, :], ckvb.t[:], start=True, stop=True), reads=[wk, ckvb], writes=[pk])
            P.op("dve", lambda e, h=h, pk=pk, cols=cols: e.tensor_tensor(KT.t[0:64, h, cols], pk.t[0:64, :], rkv_b.t[0:64, :], ALU.mult), reads=[pk, rkv_b], writes=[KT])
        for t4 in range(4):
            pv = nxt()
            P.op("pe", lambda e, t4=t4, pv=pv: e.matmul(pv.t[:], ckvb.t[:, t4 * 128:(t4 + 1) * 128], wv.t[:].rearrange("p h x -> p (h x)"), start=True, stop=True),
                 reads=[wv, ckvb], writes=[pv])
            P.op("dve", lambda e, t4=t4, pv=pv, T=T: e.tensor_scalar(VA.t[:, T * 4 + t4, :, 0:64], pv.t[:].rearrange("p (h x) -> p h x", h=8),
                                                             rkv_tok.t[:, t4:t4 + 1], None, ALU.mult), reads=[pv, rkv_tok], writes=[VA])
        tq = [(t1, t2), (t1b, t2b)]
        for h in range(8):
            pqa, pqb = nxt(), nxt()
            for a, (wq, pq_) in enumerate(((wuq, pqa), (wuqs, pqb))):
                for j in range(2):
                    P.op("pe", lambda e, j=j, h=h, wq=wq, pq_=pq_: e.matmul(pq_.t[0:96, :], wq.t[:, j, h * 96:(h + 1) * 96], cqb.t[:, j, :], start=(j == 0), stop=(j == 1)),
                         reads=[wq, cqb], writes=[pq_])
            ta, tb = tq[h % 2]
            P.op("dve", lambda e, h=h, pqa=pqa: e.tensor_tensor(QT.t[0:64, h, :], pqa.t[0:64, :], rq_b.t[0:64, :], ALU.mult), reads=[pqa, rq_b], writes=[QT])
            P.op("dve", lambda e, pqa=pqa, ta=ta: e.tensor_tensor(ta.t[R, :], pqa.t[R, :], C2r.t[R, :], ALU.mult), reads=[pqa, C2r], writes=[ta])
            P.op("dve", lambda e, pqb=pqb, tb=tb: e.tensor_tensor(tb.t[R, :], pqb.t[R, :], S2r.t[R, :], ALU.mult), reads=[pqb, S2r], writes=[tb])
            P.op("pool", lambda e, h=h, ta=ta, tb=tb: e.tensor_tensor(QT.t[R, h, :], ta.t[R, :], tb.t[R, :], ALU.add), reads=[ta, tb], writes=[QT])
        yo_ = yo[T % 2]
        blocks = []
        for h in range(8):
            nkb = 4 * T + 4
            for kb in range(nkb):
                parts = []
                if kb < 4 * T:
                    parts.append((128, 0, TT))
                else:
                    j = kb - 4 * T
                    parts.append((64, 2 * j * 64, (2 * j + 1) * 64))
                    if (2 * j + 1) * 64 < TT:
                        parts.append((128, (2 * j + 1) * 64, TT))
                blocks.append((h, kb, parts, kb == nkb - 1))
        nblk = len(blocks)
        LOOK = 2
        deferred = []

        def emit_S(i):
            h, kb, parts, _ = blocks[i]
            pS, pt = ps[2 + (nS + i) % 3], PT[(nS + i) % 3]
            for (rows, c0, c1) in parts:
                P.op("pe", lambda e, h=h, kb=kb, rows=rows, c0=c0, c1=c1, pS=pS: e.matmul(pS.t[0:rows, c0:c1], KT.t[0:96, h, kb * 128:kb * 128 + rows], QT.t[0:96, h, c0:c1],
                                                                                     start=True, stop=True), reads=[KT, QT], writes=[pS])
            for (rows, c0, c1) in parts:
                P.op("act", lambda e, rows=rows, c0=c0, c1=c1, pS=pS, pt=pt: e.activation(pt.t[0:rows, c0:c1], pS.t[0:rows, c0:c1], AF.Exp, scale=scale),
                     reads=[pS], writes=[pt])

        def emit_PV(i):
            h, kb, parts, lastkb = blocks[i]
            pt = PT[(nS + i) % 3]
            pO = ps[5 + h % 2]
            for pi, (rows, c0, c1) in enumerate(parts):
                last = lastkb and (pi == len(parts) - 1)
                P.op("pe", lambda e, h=h, kb=kb, rows=rows, c0=c0, c1=c1, pt=pt, pO=pO, last=last: e.matmul(pO.t[0:65, c0:c1], VA.t[0:rows, kb, h, 0:65], pt.t[0:rows, c0:c1],
                                                                                                start=(kb == 0), stop=last), reads=[VA, pt], writes=[pO])
            if lastkb:
                ob = Osb[h % 2]
                P.op("act", lambda e, ob=ob, pO=pO: e.activation(ob.t[0:65, :], pO.t[0:65, :], AF.Copy), reads=[pO], writes=[ob])
                deferred.append((i + LOOK, h))

        def emit_epilogue(h):
            ob = Osb[h % 2]
            P.op("pe", lambda e, ob=ob: e.matmul(ps[0].t[0:64, :], cf.t[0:65, C_SELDEN, 0:64], ob.t[0:65, :], start=True, stop=True), reads=[cf, ob], writes=[ps[0]])
            P.op("dve", lambda e: e.reciprocal(rden.t[0:64, :], ps[0].t[0:64, :]), reads=[ps[0]], writes=[rden])
            ys = yst[(h // 2) % 2]
            pb = (h % 2) * 64
            P.op("dve", lambda e, ob=ob, ys=ys, pb=pb: e.tensor_tensor(ys.t[pb:pb + 64, :], ob.t[0:64, :], rden.t[0:64, :], ALU.mult), reads=[ob, rden], writes=[ys])
            if h % 2 == 1:
                ch = h // 2
                P.op("pool", lambda e, ch=ch, ys=ys, yo_=yo_: e.tensor_tensor(yo_.t[:, ch, :], ys.t[:], sga4.t[:, ch, :], ALU.mult), reads=[ys, sga4], writes=[yo_])

        for i in range(nblk + LOOK):
            if i < nblk:
                emit_S(i)
            j = i - LOOK
            if j >= 0:
                emit_PV(j)
            while deferred and deferred[0][0] <= j:
                emit_epilogue(deferred.pop(0)[1])
        while deferred:
            emit_epilogue(deferred.pop(0)[1])
        nS += nblk
        P.dma("pool", c.yT[0].t[:, :, cols], yo_.t[:], reads=[yo_], writes=[c.ya_views[T]], semkey=K(yo_))
    P.release(m0)


def stage_mlstm(c):
    P = c.P
    l = c.l
    m0 = P.mark()
    ps = c.ps
    cf = c.cf
    NB_ = 2056
    wB = P.sbuf("wB", [128, KC, NB_], BF16)
    for kc in range(KC):
        P.dma("pool", wB.t[:, kc, :], c.w_in[l, kc * 128:(kc + 1) * 128, O_MQ:O_MQ + NB_], writes=[wB], semkey="wB")
    if c.after_w is not None:
        c.after_w()
    gmh = P.sbuf("gmh", [128, 512], F32)
    P.dma("sp", gmh.t[:], c.rows[l, R_GMH:R_GMH + 1, 0:512].to_broadcast([128, 512]), writes=[gmh], semkey="gmh")
    fbb = P.sbuf("fbb", [128, 4], F32)
    P.dma("sp", fbb.t[:], c.rows[l, R_FB:R_FB + 1, 0:4].to_broadcast([128, 4]), writes=[fbb], semkey="fbb")
    hTb = [P.sbuf(f"bhT{i}", [128, KC, TT], BF16) for i in range(2)]
    qT = P.sbuf("bqT", [64, 4, TT], BF16)
    qz = [P.sbuf(f"bqz{i}", [64, 4, TT], BF16) for i in range(2)]
    kT = P.sbuf("bkT", [64, 4, TT], BF16)
    ktok = P.sbuf("bktok", [128, 4, 4, 64], BF16)
    vtmp = P.sbuf("bvtmp", [128, 4, 512], F32)
    graw = P.sbuf("bgraw", [128, 4, 8], F32)
    fp = P.sbuf("bfp", [128, 4, 4], F32)
    l1 = P.sbuf("bl1", [128, 4, 4], F32)
    eb = P.sbuf("beb", [128, 4, 4], F32)
    ui = P.sbuf("bui", [128, 4, 4], F32)
    uu = P.sbuf("buu", [128, 4, 4], F32)
    egr = [P.sbuf(f"begr{i}", [64, 2, 4, 4], F32) for i in range(2)]
    vp = P.sbuf("bvp", [128, 4, 4, 136], BF16)
    Usb = P.sbuf("bUsb", [64, 8, 4, 129], F32)
    Z = P.sbuf("bZ", [64, 4, 129], F32)
    Cb = [P.sbuf(f"bCb{i}", [64, 4, 136], BF16) for i in range(8)]
    STm = [P.sbuf(f"bSTm{i}", [128, 4, 128], BF16) for i in range(4)]
    dn = P.sbuf("bdn", [128, 4], F32)
    rr = P.sbuf("brr", [128, 4], F32)
    hh = P.sbuf("bhh", [128, 4, 128], F32)
    sq = P.sbuf("bsq", [128, 4, 128], F32)
    ssm = P.sbuf("bssm", [128, 4], F32)
    so4 = [P.sbuf(f"bso{i}", [128, 512], F32) for i in range(4)]
    sg = P.sbuf("bsg", [128, 512], F32)
    ybt = [P.sbuf(f"bybt{i}", [128, 512], BF16) for i in range(2)]
    yo = [P.sbuf(f"byo{i}", [128, 4, TT], BF16) for i in range(2)]
    c.yb_views = [P.view(f"ybv{i}") for i in range(NT)]
    for i in range(2):
        P.op("pool", lambda e, i=i: e.memset(qz[i].t[:], 0.0), writes=[qz[i]])
    P.op("pool", lambda e: e.memset(vp.t[:], 0.0), writes=[vp])
    for i in range(8):
        P.op("pool", lambda e, i=i: e.memset(Cb[i].t[:], 0.0), writes=[Cb[i]])
    load_hT(c, hTb[0], 0)
    for T in range(NT):
        cols = slice(T * TT, (T + 1) * TT)
        h_ = hTb[T % 2]
        if T + 1 < NT:
            load_hT(c, hTb[(T + 1) % 2], T + 1)
        eg_cur, eg_prev = egr[T % 2], egr[(T + 1) % 2]
        for h in range(4):
            pq = ps[h % 2]
            proj_T(c, pq, lambda kc, h=h: wB.t[:, kc, h * 64:(h + 1) * 64], h_, wB, M=64)
            P.op("act", lambda e, h=h, pq=pq: e.activation(qT.t[0:64, h, :], pq.t[0:64, :], AF.Copy, scale=0.125), reads=[pq], writes=[qT])
            for par in range(2):
                P.op("pool", lambda e, h=h, par=par: e.tensor_copy(qz[par].t[0:64, h, :].rearrange("p (a b x) -> p a b x", a=4, b=2)[:, :, par, :],
                                                                  qT.t[0:64, h, :].rearrange("p (a b x) -> p a b x", a=4, b=2)[:, :, par, :]),
                     reads=[qT], writes=[qz[par]])
        for h in range(4):
            pk = ps[h % 2]
            proj_T(c, pk, lambda kc, h=h: wB.t[:, kc, 256 + h * 64:256 + (h + 1) * 64], h_, wB, M=64)
            P.op("act", lambda e, h=h, pk=pk: e.activation(kT.t[0:64, h, :], pk.t[0:64, :], AF.Copy), reads=[pk], writes=[kT])
        for t4 in range(4):
            pA, pBk = ps[2], ps[3]
            for kc in range(KC):
                P.op("pe", lambda e, kc=kc, t4=t4, h_=h_: e.matmul(pA.t[:], h_.t[:, kc, t4 * 128:(t4 + 1) * 128], wB.t[:, kc, 256:768], start=(kc == 0), stop=(kc == KC - 1)),
                     reads=[h_, wB], writes=[pA])
            for kc in range(KC):
                P.op("pe", lambda e, kc=kc, t4=t4, h_=h_: e.matmul(pBk.t[:, 0:264], h_.t[:, kc, t4 * 128:(t4 + 1) * 128], wB.t[:, kc, 768:1032], start=(kc == 0), stop=(kc == KC - 1)),
                     reads=[h_, wB], writes=[pBk])
            P.op("act", lambda e, t4=t4: e.activation(ktok.t[:, t4, :, :].rearrange("p h x -> p (h x)"), pA.t[:, 0:256], AF.Copy), reads=[pA], writes=[ktok])
            P.op("act", lambda e, t4=t4: e.activation(vtmp.t[:, t4, 0:256], pA.t[:, 256:512], AF.Copy), reads=[pA], writes=[vtmp])
            P.op("dve", lambda e, t4=t4: e.tensor_copy(vtmp.t[:, t4, 256:512], pBk.t[:, 0:256]), reads=[pBk], writes=[vtmp])
            P.op("dve", lambda e, t4=t4: e.tensor_copy(graw.t[:, t4, :], pBk.t[:, 256:264]), reads=[pBk], writes=[graw])

        def og_tiles(tiles):
            for t4 in tiles:
                tc_ = slice(t4 * 128, (t4 + 1) * 128)
                pO, pG = ps[2], ps[3]
                so = so4[t4]
                for kc in range(KC):
                    P.op("pe", lambda e, kc=kc, tc_=tc_, h_=h_: e.matmul(pO.t[:], h_.t[:, kc, tc_], wB.t[:, kc, 1032:1544], start=(kc == 0), stop=(kc == KC - 1)), reads=[h_, wB], writes=[pO])
                for kc in range(KC):
                    P.op("pe", lambda e, kc=kc, tc_=tc_, h_=h_: e.matmul(pG.t[:], h_.t[:, kc, tc_], wB.t[:, kc, 1544:2056], start=(kc == 0), stop=(kc == KC - 1)), reads=[h_, wB], writes=[pG])
                P.op("act", lambda e, so=so: e.activation(so.t[:], pO.t[:], AF.Sigmoid), reads=[pO], writes=[so])
                P.op("act", lambda e: e.activation(sg.t[:], pG.t[:], AF.Silu), reads=[pG], writes=[sg])
                P.op("pool", lambda e, so=so: e.tensor_tensor(so.t[:], so.t[:], sg.t[:], ALU.mult), reads=[so, sg], writes=[so])
                P.op("pool", lambda e, so=so: e.tensor_tensor(so.t[:], so.t[:], gmh.t[:], ALU.mult), reads=[so, gmh], writes=[so])

        P.op("dve", lambda e: e.tensor_tensor(fp.t[:], graw.t[:, :, 4:8], fbb.t[:].unsqueeze(1).to_broadcast([128, 4, 4]), ALU.add), reads=[graw, fbb], writes=[fp])
        P.op("act", lambda e: e.activation(l1.t[:], fp.t[:], AF.Exp, scale=-1.0), reads=[fp], writes=[l1])
        P.op("act", lambda e: e.activation(l1.t[:], l1.t[:], AF.Ln, bias=1.0), reads=[l1], writes=[l1])
        og_tiles((0, 1))
        l1f = l1.t[:].rearrange("p a b -> p (a b)")
        pg = ps[6]
        P.op("pe", lambda e: e.matmul(pg.t[:, 0:16], cf.t[:, C_TRI, :], l1f, start=True, stop=True), reads=[cf, l1], writes=[pg])
        for par in range(2):
            P.op("pe", lambda e, par=par: e.matmul(pg.t[0:64, 16 + par * 16:32 + par * 16], cf.t[:, C_SEL0 + par, 0:64], l1f, start=True, stop=True),
                 reads=[cf, l1], writes=[pg])
        nb3 = pg.t[:, 0:16].rearrange("p (a b) -> p a b", a=4)
        P.op("act", lambda e: e.activation(eb.t[:], nb3, AF.Exp, scale=-1.0), reads=[pg], writes=[eb])
        P.op("dve", lambda e: e.tensor_tensor(ui.t[:], nb3, graw.t[:, :, 0:4], ALU.add), reads=[pg, graw], writes=[ui])
        P.op("act", lambda e: e.activation(uu.t[:], ui.t[:], AF.Exp), reads=[ui], writes=[uu])
        P.op("act", lambda e, eg_cur=eg_cur: e.activation(eg_cur.t[:].rearrange("p a b c -> p (a b c)"), pg.t[0:64, 16:48], AF.Exp, scale=-1.0), reads=[pg], writes=[eg_cur])
        og_tiles((2, 3))
        P.op("dve", lambda e: e.tensor_tensor(vp.t[:, :, :, 0:128], vtmp.t[:].rearrange("p a (h x) -> p a h x", h=4),
                                              uu.t[:].unsqueeze(3).to_broadcast([128, 4, 4, 128]), ALU.mult), reads=[vtmp, uu], writes=[vp])
        P.op("dve", lambda e: e.tensor_copy(vp.t[:, :, :, 128:129], uu.t[:].unsqueeze(3)), reads=[uu], writes=[vp])
        for cidx in range(8):
            t4, par = cidx // 2, cidx % 2
            rs_ = slice(par * 64, par * 64 + 64)
            for hp in range(2):
                pu = ps[hp]
                for hh_ in range(2):
                    h = hp * 2 + hh_
                    P.op("pe", lambda e, h=h, hh_=hh_, t4=t4, rs_=rs_, pu=pu: e.matmul(pu.t[0:64, hh_ * 129:(hh_ + 1) * 129], ktok.t[rs_, t4, h, :], vp.t[rs_, t4, h, 0:129],
                                                                                start=True, stop=True), reads=[ktok, vp], writes=[pu])
                P.op("act", lambda e, hp=hp, cidx=cidx, pu=pu: e.activation(Usb.t[0:64, cidx, 2 * hp:2 * hp + 2, :].rearrange("p h x -> p (h x)"), pu.t[0:64, 0:258], AF.Copy),
                     reads=[pu], writes=[Usb])
        for t4 in range(4):
            tc_ = slice(t4 * 128, (t4 + 1) * 128)
            pS = ps[6] if t4 % 2 == 0 else ps[0]
            stm = STm[t4]
            for h in range(4):
                P.op("pe", lambda e, h=h, tc_=tc_, pS=pS: e.matmul(pS.t[:, h * 128:(h + 1) * 128], kT.t[0:64, h, tc_], qT.t[0:64, h, tc_], start=True, stop=True),
                     reads=[kT, qT], writes=[pS])
            P.op("dve", lambda e, stm=stm, pS=pS: e.tensor_tensor(stm.t[:], pS.t[:].rearrange("p (h t) -> p h t", h=4),
                                                          cf.t[:, C_TRI:C_TRI + 1, :].to_broadcast([128, 4, 128]), ALU.mult), reads=[pS, cf], writes=[stm])
        for cidx in range(8):
            gc = T * 8 + cidx
            if gc == 0:
                P.op("dve", lambda e: e.tensor_copy(Z.t[:], Usb.t[0:64, 0, :, :]), reads=[Usb], writes=[Z])
                continue
            pc = cidx - 1
            if pc >= 0:
                egp = eg_cur.t[0:64, pc % 2, pc // 2, :]
                egb_ = eg_cur
            else:
                egp = eg_prev.t[0:64, 1, 3, :]
                egb_ = eg_prev
            egp3 = egp.unsqueeze(2).to_broadcast([64, 4, 129])
            P.op("pool", lambda e, cidx=cidx, egp3=egp3: e.tensor_tensor(Cb[cidx].t[0:64, :, 0:129], Z.t[:], egp3, ALU.mult), reads=[Z, egb_], writes=[Cb[cidx]])
            P.op("dve", lambda e, egp3=egp3: e.tensor_tensor(Z.t[:], Z.t[:], egp3, ALU.mult), reads=[Z, egb_], writes=[Z])
            P.op("dve", lambda e, cidx=cidx: e.tensor_tensor(Z.t[:], Z.t[:], Usb.t[0:64, cidx, :, :], ALU.add), reads=[Z, Usb], writes=[Z])
        yo_ = yo[T % 2]

        def nd_banks(t4):
            return (ps[4], ps[5]) if t4 % 2 == 0 else (ps[2], ps[3])

        def nd_mm(t4):
            tc_ = slice(t4 * 128, (t4 + 1) * 128)
            stm = STm[t4]
            bk = nd_banks(t4)
            for h in range(4):
                pn = bk[h // 2]
                o0 = (h % 2) * 129
                P.op("pe", lambda e, h=h, t4=t4, pn=pn, o0=o0, stm=stm: e.matmul(pn.t[:, o0:o0 + 129], stm.t[:, h, :], vp.t[:, t4, h, 0:129], start=True, stop=False),
                     reads=[stm, vp], writes=[pn])
                for par in range(2):
                    cb_ = Cb[2 * t4 + par]
                    P.op("pe", lambda e, h=h, par=par, pn=pn, o0=o0, cb_=cb_, tc_=tc_: e.matmul(pn.t[:, o0:o0 + 129], qz[par].t[0:64, h, tc_], cb_.t[0:64, h, 0:129],
                                                                                          start=False, stop=(par == 1)), reads=[qz[par], cb_], writes=[pn])

        def out_chain(t4):
            bk = nd_banks(t4)
            for b2 in range(2):
                pn = bk[b2]
                P.op("dve", lambda e, pn=pn, b2=b2, t4=t4: e.tensor_tensor(dn.t[:, 2 * b2:2 * b2 + 2], pn.t[:, 0:258].rearrange("p (h x) -> p h x", h=2)[:, :, 128],
                                                                        eb.t[:, t4, 2 * b2:2 * b2 + 2], ALU.mult), reads=[pn, eb], writes=[dn])
            P.op("dve", lambda e: e.tensor_scalar(rr.t[:], dn.t[:], -1.0, None, ALU.mult), reads=[dn], writes=[rr])
            P.op("dve", lambda e: e.tensor_tensor(dn.t[:], dn.t[:], rr.t[:], ALU.max), reads=[dn, rr], writes=[dn])
            P.op("dve", lambda e: e.tensor_scalar_max(dn.t[:], dn.t[:], 1.0), reads=[dn], writes=[dn])
            P.op("dve", lambda e: e.reciprocal(dn.t[:], dn.t[:]), reads=[dn], writes=[dn])
            P.op("dve", lambda e, t4=t4: e.tensor_tensor(rr.t[:], eb.t[:, t4, :], dn.t[:], ALU.mult), reads=[eb, dn], writes=[rr])
            for b2 in range(2):
                pn = bk[b2]
                P.op("dve", lambda e, pn=pn, b2=b2: e.tensor_tensor(hh.t[:, 2 * b2:2 * b2 + 2, :], pn.t[:, 0:258].rearrange("p (h x) -> p h x", h=2)[:, :, 0:128],
                                                                  rr.t[:, 2 * b2:2 * b2 + 2].unsqueeze(2).to_broadcast([128, 2, 128]), ALU.mult), reads=[pn, rr], writes=[hh])

        def out_chain2(t4):
            tc_ = slice(t4 * 128, (t4 + 1) * 128)
            P.op("pool", lambda e: e.tensor_tensor(sq.t[:], hh.t[:], hh.t[:], ALU.mult), reads=[hh], writes=[sq])
            P.op("dve", lambda e: e.tensor_reduce(ssm.t[:], sq.t[:], AX.X, ALU.add), reads=[sq], writes=[ssm])
            rstd_from_ss(P, ssm.t[:], ssm.t[:], 128, [ssm], [ssm])
            P.op("dve", lambda e: e.tensor_tensor(hh.t[:], hh.t[:], ssm.t[:].unsqueeze(2).to_broadcast([128, 4, 128]), ALU.mult), reads=[hh, ssm], writes=[hh])
            so = so4[t4]
            yb_ = ybt[t4 % 2]
            P.op("dve", lambda e, yb_=yb_, so=so: e.tensor_tensor(yb_.t[:], hh.t[:].rearrange("p h x -> p (h x)"), so.t[:], ALU.mult), reads=[hh, so], writes=[yb_])
            for ch in range(4):
                P.op("pe", lambda e, ch=ch, yb_=yb_: e.transpose(c.pst.t[:, ch * 128:(ch + 1) * 128], yb_.t[:, ch * 128:(ch + 1) * 128], c.ident.t[:]),
                     reads=[yb_, c.ident], writes=[c.pst])
            P.op("act", lambda e, yo_=yo_, tc_=tc_: e.activation(yo_.t[:, :, tc_], c.pst.t[:, 0:512].rearrange("p (a t) -> p a t", a=4), AF.Copy), reads=[c.pst], writes=[yo_])

        nd_mm(0)
        nd_mm(1)
        for t4 in range(4):
            out_chain(t4)
            if t4 + 2 < 4:
                nd_mm(t4 + 2)
            out_chain2(t4)
        P.dma("sp", c.yT[1].t[:, :, cols], yo_.t[:], reads=[yo_], writes=[c.yb_views[T]], semkey=K(yo_))
    P.release(m0)


def make_consts():
    s = np.arange(128)[:, None]
    t = np.arange(128)[None, :]
    same = (s // 64) == (t // 64)
    cm = np.zeros((NCONST, 128, 128), np.float32)
    cm[C_IDENT] = np.eye(128, dtype=np.float32)
    cm[C_TRI] = ((s <= t) & same)
    cm[C_BLK] = same
    cm[C_TRIL128] = (s <= t)
    cm[C_SEL0] = np.broadcast_to(s < 64, (128, 128))
    cm[C_SEL1] = np.broadcast_to(s >= 64, (128, 128))
    cm[C_ONES] = 1.0
    cm[C_SELDEN] = np.broadcast_to(s == 64, (128, 128))
    half = 16
    inv = (np.float32(10000.0) ** (-np.arange(half, dtype=np.float32) / np.float32(half))).astype(np.float32)
    invf = np.zeros((128, 4), np.float32)
    r = np.arange(128)
    invf[:, 0] = inv[r % 16]
    first = (r % 32) < 16
    invf[:, 1] = np.where(first, -TWO_PI, TWO_PI)
    invf[:, 2] = np.where(first, -np.pi, np.pi)
    invf[:, 3] = np.pi
    return cm, invf


def pack_weights(inp, n_layers, l0=0):
    sl = slice(l0, l0 + n_layers)
    f = lambda k: np.ascontiguousarray(np.asarray(inp[k], np.float32)[sl])
    w_in = f("w_in")
    w_uq = f("w_uq")
    L = n_layers
    w_uq_sw = np.zeros_like(w_uq)
    for h in range(8):
        b0 = h * 96 + 64
        w_uq_sw[:, :, b0:b0 + 16] = w_uq[:, :, b0 + 16:b0 + 32]
        w_uq_sw[:, :, b0 + 16:b0 + 32] = w_uq[:, :, b0:b0 + 16]
    w_kr = np.zeros((L, D, 2, 96), np.float32)
    w_kr[:, :, 0, 64:96] = w_in[:, :, O_KR:O_KR + 32]
    w_kr[:, :, 1, 64:80] = w_in[:, :, O_KR + 16:O_KR + 32]
    w_kr[:, :, 1, 80:96] = w_in[:, :, O_KR:O_KR + 16]
    w_p = np.stack([f("w_pa"), f("w_pb"), f("w_pc"), f("w_pd")], axis=1)
    w_sT = np.ascontiguousarray(np.transpose(f("w_s"), (0, 1, 3, 2)))
    vecs = np.zeros((L, 128, NVEC), np.float32)
    vecs[:, :, V_GPRE:V_GPRE + 8] = f("g_pre").reshape(L, 8, 128).transpose(0, 2, 1)
    wc = f("w_conv").reshape(L, 3, 4, 128)
    vecs[:, :, V_WCONV:V_WCONV + 12] = wc.transpose(0, 3, 2, 1).reshape(L, 128, 12)
    vecs[:, :, V_BCONV:V_BCONV + 4] = f("b_conv").reshape(L, 4, 128).transpose(0, 2, 1)
    vecs[:, :, V_GSV:V_GSV + 4] = f("g_sv").reshape(L, 4, 128).transpose(0, 2, 1)
    vecs[:, :, V_BGATE:V_BGATE + 32] = f("b_gate").reshape(L, 32, 128).transpose(0, 2, 1)
    vecs[:, :, V_GCQ:V_GCQ + 2] = f("g_cq").reshape(L, 2, 128).transpose(0, 2, 1)
    vecs[:, :, V_GCKV:V_GCKV + 1] = f("g_ckv").reshape(L, 1, 128).transpose(0, 2, 1)
    rows = np.zeros((L, NROW, 1024), np.float32)
    rows[:, R_GPOST, :] = f("g_post")
    rows[:, R_GMH, :512] = f("g_mh")
    rows[:, R_BS, :512] = f("b_s").reshape(L, 512)
    rows[:, R_FB, :4] = f("f_bias")
    cm, invf = make_consts()
    return {
        "w_in": w_in, "w_gate": f("w_gate"), "w_uq": w_uq, "w_uq_sw": w_uq_sw, "w_ukv": f("w_ukv"), "w_kr": w_kr,
        "w_p": w_p, "w_out": f("w_out"), "w_sT": w_sT, "vecs": vecs, "rows": rows, "consts": cm, "inv_freq": invf,
    }


STAGE_FNS = (("attn", lambda c: stage_attn(c), "ya_views"), ("mlstm", lambda c: stage_mlstm(c), "yb_views"),
             ("conv", lambda c: stage_conv(c), "yc_views"), ("sgu", lambda c: stage_sgu(c), "yd_views"))

_PROG = {}


def get_prog(n_layers):
    if n_layers not in _PROG:
        _PROG[n_layers] = build_program(n_layers)
    return _PROG[n_layers]


N_LAUNCH_LAYERS = 2


def kernel(**inp):
    x = np.asarray(inp["x"], np.float32)
    pos = np.asarray(inp["positions"], np.int32)
    B = x.shape[0]
    cur = x
    for l0 in range(0, NL, N_LAUNCH_LAYERS):
        nc, _ = get_prog(N_LAUNCH_LAYERS)
        w = pack_weights(inp, N_LAUNCH_LAYERS, l0)
        in_maps = []
        for b in range(B):
            m = dict(w)
            m["x"] = np.ascontiguousarray(cur[b])
            m["pos"] = np.ascontiguousarray(pos[b][None, :])
            in_maps.append(m)
        res = run_bass_kernel_spmd(nc, in_maps, core_ids=list(range(B)))
        cur = np.stack([np.asarray(r["out"], np.float32) for r in res.results], axis=0)
    return cur
```

```python
import numpy as np
import concourse.bass as bass
import concourse.mybir as mybir
from concourse.bass_utils import run_bass_kernel_spmd

F32 = mybir.dt.float32
BF16 = mybir.dt.bfloat16
I32 = mybir.dt.int32
AF = mybir.ActivationFunctionType
ALU = mybir.AluOpType
AX = mybir.AxisListType


class Buf:
    __slots__ = ("name", "t", "last_w", "readers")

    def __init__(self, name, t=None):
        self.name = name
        self.t = t
        self.last_w = None
        self.readers = []


class Op:
    __slots__ = ("eng", "fn", "deps", "is_dma", "ndma", "semkey", "signal", "sem", "val", "gidx")

    def __init__(self, eng, fn, is_dma=False, ndma=1, semkey=None):
        self.eng = eng
        self.fn = fn
        self.deps = []
        self.is_dma = is_dma
        self.ndma = ndma
        self.semkey = semkey
        self.signal = False
        self.sem = None
        self.val = 0
        self.gidx = 0


class Planner:
    ENGS = ("pe", "act", "dve", "pool", "sp")
    SEM_EPOCH = 30000

    def __init__(self, nc):
        self.nc = nc
        self.ops = {e: [] for e in self.ENGS}
        self.all_ops = []
        self.last_dma = {}
        self.fence = []
        self.sb_off = int(nc.sbuf_base)
        self.sb_hi = 0
        self.sb_top = int(nc.sbuf_top)
        self.top_off = self.sb_top
        self.n_names = 0
        self.psum_bufs = []
        self.sbuf_base = None

    def _dsize(self, dtype):
        return {F32: 4, BF16: 2, I32: 4}[dtype]

    def sbuf(self, name, shape, dtype, align=64, top=False):
        per_part = int(np.prod(shape[1:])) * self._dsize(dtype)
        if top:
            off = (self.top_off - per_part) // align * align
            assert off >= self.sb_off, f"SBUF overflow (top) allocating {name}"
            self.n_names += 1
            t = self.nc.alloc_sbuf_tensor_at(f"{name}_{self.n_names}", list(shape), dtype, offset=off)
            self.top_off = off
            return Buf(name, t)
        off = (self.sb_off + align - 1) // align * align
        self.n_names += 1
        t = self.nc.alloc_sbuf_tensor_at(f"{name}_{self.n_names}", list(shape), dtype, offset=off)
        self.sb_off = off + per_part
        assert self.sb_off <= self.top_off, f"SBUF overflow allocating {name}: {self.sb_off} > {self.top_off}"
        self.sb_hi = max(self.sb_hi, self.sb_off)
        return Buf(name, t)

    def mark(self):
        return self.sb_off

    def free_top(self):
        self.top_off = self.sb_top

    def release(self, mark):
        self.sb_off = mark
        self.barrier()

    def psum(self, name, shape, dtype=F32):
        self.n_names += 1
        t = self.nc.alloc_psum_tensor(f"{name}_{self.n_names}", list(shape), dtype)
        return Buf(name, t)

    def dram(self, name, shape, dtype, kind="Internal"):
        t = self.nc.dram_tensor(name, list(shape), dtype, kind=kind)
        return Buf(name, t)

    def view(self, name="v"):
        return Buf(name, None)

    def _add(self, o, reads, writes):
        deps = {}
        for b in reads:
            if b.last_w is not None:
                deps[id(b.last_w)] = b.last_w
        for b in writes:
            if b.last_w is not None:
                deps[id(b.last_w)] = b.last_w
            for r in b.readers:
                deps[id(r)] = r
        for f in self.fence:
            deps[id(f)] = f
        deps.pop(id(o), None)
        o.deps = list(deps.values())
        for b in writes:
            b.last_w = o
            b.readers = []
        for b in reads:
            b.readers.append(o)
        o.gidx = len(self.all_ops)
        self.all_ops.append(o)
        self.ops[o.eng].append(o)
        return o

    def op(self, eng, fn, reads=(), writes=()):
        return self._add(Op(eng, fn), reads, writes)

    def dma(self, eng, out_ap, in_ap, reads=(), writes=(), semkey=None, **kw):
        if semkey is None:
            semkey = (writes[0].name if writes else reads[0].name) + "#" + str(id(writes[0] if writes else reads[0]))
        o = Op(eng, lambda e: e.dma_start(out=out_ap, in_=in_ap, **kw), is_dma=True, ndma=1, semkey=semkey)
        prev = self.last_dma.get(semkey)
        self._add(o, reads, writes)
        if prev is not None and all(d is not prev for d in o.deps):
            o.deps.append(prev)
        self.last_dma[semkey] = o
        return o

    def barrier(self):
        f = []
        for e in self.ENGS:
            for o in reversed(self.ops[e]):
                if not o.is_dma:
                    f.append(o)
                    break
        f.extend(self.last_dma.values())
        self.fence = f

    def finish(self):
        nc = self.nc
        for o in self.all_ops:
            for d in o.deps:
                if d.is_dma or not (d.eng == "pe" and o.eng == "pe" and not o.is_dma):
                    d.signal = True
        for o in self.last_dma.values():
            o.signal = True
        eng_sems = {}
        cnt = {}
        dma_sems = {}
        dma_cnt = {}
        final_dma = {}
        for o in self.all_ops:
            if o.is_dma:
                o.signal = True
                if o.semkey not in dma_sems:
                    dma_sems[o.semkey] = nc.alloc_semaphore(f"d{len(dma_sems)}")
                    dma_cnt[o.semkey] = 0
                dma_cnt[o.semkey] += 16 * o.ndma
                o.sem = dma_sems[o.semkey]
                o.val = dma_cnt[o.semkey]
                final_dma[o.semkey] = o
            elif o.signal:
                e = o.eng
                if e not in eng_sems or cnt[e] >= self.SEM_EPOCH:
                    eng_sems[e] = nc.alloc_semaphore(f"e_{e}_{len(eng_sems)}_{o.gidx}")
                    cnt[e] = 0
                cnt[e] += 1
                o.sem = eng_sems[e]
                o.val = cnt[e]
        self.n_sems = len(dma_sems)
        ops = self.ops
        final_list = list(final_dma.values())

        def emit(engname, eng):
            known = {}
            for o in ops[engname]:
                for d in o.deps:
                    if not d.signal:
                        continue
                    if (not d.is_dma) and d.eng == "pe" and engname == "pe" and not o.is_dma:
                        continue
                    k = id(d.sem)
                    if known.get(k, 0) >= d.val:
                        continue
                    eng.wait_ge(d.sem, d.val)
                    known[k] = d.val
                ins = o.fn(eng)
                if o.signal:
                    if o.is_dma:
                        ins.then_inc(o.sem, 16)
                    else:
                        ins.then_inc(o.sem, 1)
            if engname == "sp":
                for o in final_list:
                    k = id(o.sem)
                    if known.get(k, 0) >= o.val:
                        continue
                    eng.wait_ge(o.sem, o.val)
                    known[k] = o.val

        with nc.Block() as block:
            block.tensor(lambda e: emit("pe", e))
            block.scalar(lambda e: emit("act", e))
            block.vector(lambda e: emit("dve", e))
            block.gpsimd(lambda e: emit("pool", e))
            block.sync(lambda e: emit("sp", e))


D = 1024
S = 4096
NL = 2
EPS = 1e-6
TT = 512
NT = S // TT
KC = D // 128
IN_SIZES = (256, 128, 32, 512, 256, 256, 512, 4, 4, 512, 512, 512, 512, 512, 512, 1024, 512)
IN_OFF = [0]
for _s in IN_SIZES:
    IN_OFF.append(IN_OFF[-1] + _s)
(O_CQ, O_CKV, O_KR, O_GA, O_MQ, O_MK, O_MV, O_MI, O_MF, O_MO, O_GB,
 O_CX, O_CB, O_CC, O_GC, O_DUV, O_GD) = IN_OFF[:17]
IN_DIM = IN_OFF[-1]
TWO_PI = 6.283185307179586


class Ctx:
    pass


def _bc(ap, shape):
    return ap.to_broadcast(list(shape))


ALL_STAGES = ("attn", "mlstm", "conv", "sgu")


def build_program(n_layers, dbg=False, stages=ALL_STAGES):
    nc = bass.Bass("TRN2", target_bir_lowering=False)
    P = Planner(nc)
    c = Ctx()
    c.nc, c.P, c.dbg = nc, P, dbg

    def din(name, shape, dtype=F32):
        return nc.dram_tensor(name, list(shape), dtype, kind="ExternalInput").ap()

    c.x = din("x", [S, D])
    c.pos = din("pos", [1, S], I32)
    c.w_in = din("w_in", [n_layers, D, IN_DIM])
    c.w_gate = din("w_gate", [n_layers, D, 4 * D])
    c.w_uq = din("w_uq", [n_layers, 256, 768])
    c.w_uq_sw = din("w_uq_sw", [n_layers, 256, 768])
    c.w_ukv = din("w_ukv", [n_layers, 128, 1024])
    c.w_kr = din("w_kr", [n_layers, D, 2, 96])
    c.w_p = din("w_p", [n_layers, 4, 512, D])
    c.w_out = din("w_out", [n_layers, D, D])
    c.w_sT = din("w_sT", [n_layers, 4, 128, 128])
    c.vecs = din("vecs", [n_layers, 128, NVEC])
    c.rows = din("rows", [n_layers, NROW, 1024])
    c.consts = din("consts", [NCONST, 128, 128])
    c.inv_freq = din("inv_freq", [128, 4])
    c.out = nc.dram_tensor("out", [S, D], F32, kind="ExternalOutput").ap()
    c.hT = P.dram("hT_s", [128, KC, S], BF16)
    c.yT = [P.dram(f"yT_s{b}", [128, 4, S], BF16, kind=("ExternalOutput" if dbg else "Internal")) for b in range(4)]
    c.xmid = P.dram("xmid_s", [S, D], F32)
    c.xmid_views = [P.view(f"xmv{i}") for i in range(S // 128)]
    c.rope_tab = P.dram("rope_s", [2, 32, S], F32)
    for b_ in [c.hT, c.xmid, c.rope_tab] + c.yT:
        b_.t = b_.t.ap()
    c.ps = [P.psum(f"ps{i}", [128, 512], F32) for i in range(7)]
    c.pst = P.psum("pst", [128, 1024], BF16)
    c.ident = P.sbuf("ident", [128, 128], BF16)
    P.dma("pool", c.ident.t[:], c.consts[0], writes=[c.ident], semkey="const")
    c.cf = P.sbuf("cf", [128, NCONST, 128], F32)
    P.dma("sp", c.cf.t[:], c.consts.rearrange("n p f -> p n f"), writes=[c.cf], semkey="const2")
    c.cb = P.sbuf("cb", [128, NCONST, 128], BF16)
    P.dma("pool", c.cb.t[:], c.consts.rearrange("n p f -> p n f"), writes=[c.cb], semkey="const")

    stage_setup(c)
    for l in range(n_layers):
        c.l = l
        c.x_in = c.x if l == 0 else c.xmid.t
        c.x_in_buf = None if l == 0 else c.xmid
        last = (l == n_layers - 1)
        c.x_out = c.out if last else c.xmid.t
        c.x_out_buf = None if last else c.xmid
        m0 = P.mark()
        c.vec = P.sbuf("vec", [128, NVEC], F32)
        P.dma("sp", c.vec.t[:], c.vecs[l], writes=[c.vec], semkey="vec")
        if "attn" in stages:
            c.Wattn = w_attn(c)
        stage_h(c)
        if "attn" in stages:
            P.release(c.attn_stg_mark)
        c.yT_src = [None] * 4
        for bi, (nm, fn, vn) in enumerate(STAGE_FNS):
            if nm in stages:
                c.after_w = None
                if nm == "mlstm" and "conv" in stages:
                    c.after_w = lambda: setattr(c, "Wconv", w_conv(c))
                if nm == "conv" and "mlstm" not in stages:
                    c.Wconv = w_conv(c)
                if nm == "sgu":
                    c.after_w = lambda: setattr(c, "Wfinal", w_final(c))
                fn(c)
                if nm in ("attn", "conv"):
                    P.free_top()
                c.yT_src[bi] = c.yT[bi].t
            else:
                c.yT_src[bi] = din(f"yTin{bi}_{l}", [128, 4, S], BF16)
                setattr(c, vn, [P.view(f"{vn}{i}") for i in range(NT)])
        if "sgu" not in stages:
            c.Wfinal = w_final(c)
        stage_final(c)
        P.free_top()
        P.release(m0)
    P.finish()
    return nc, P


V_GPRE = 0
V_WCONV = 8
V_BCONV = 20
V_GSV = 24
V_BGATE = 28
V_GCQ = 60
V_GCKV = 62
NVEC = 64
R_GPOST = 0
R_GMH = 1
R_BS = 2
R_FB = 3
NROW = 4
C_IDENT = 0
C_TRI = 1
C_BLK = 2
C_TRIL128 = 3
C_SEL0 = 4
C_SEL1 = 5
C_ONES = 6
C_SELDEN = 7
NCONST = 8


def K(buf):
    return buf.name


def stage_setup(c):
    P = c.P
    m0 = P.mark()
    invf = P.sbuf("invf", [128, 4], F32)
    P.dma("sp", invf.t[:], c.inv_freq, writes=[invf], semkey="invf")
    posi = P.sbuf("posi", [32, S], I32)
    P.dma("sp", posi.t[:], c.pos.to_broadcast([32, S]), writes=[posi], semkey="posi")
    ang = P.sbuf("ang", [32, S], F32)
    P.op("dve", lambda e: e.tensor_copy(ang.t[:], posi.t[:]), reads=[posi], writes=[ang])
    P.op("dve", lambda e: e.tensor_scalar(ang.t[:], ang.t[:], invf.t[0:32, 0:1], None, ALU.mult), reads=[ang, invf], writes=[ang])
    ki = P.sbuf("angk", [32, S], I32)
    tmp = P.sbuf("angt", [32, S], F32)
    tmp2 = P.sbuf("angt2", [32, S], F32)
    tab = P.sbuf("tab", [32, S], F32)

    def frac_sin(off, scale_arg, slot, key):
        P.op("dve", lambda e: e.tensor_scalar(tmp.t[:], ang.t[:], float(1.0 / TWO_PI), float(off), ALU.mult, ALU.add), reads=[ang], writes=[tmp])
        P.op("dve", lambda e: e.tensor_copy(ki.t[:], tmp.t[:]), reads=[tmp], writes=[ki])
        P.op("dve", lambda e: e.tensor_copy(tmp2.t[:], ki.t[:]), reads=[ki], writes=[tmp2])
        P.op("dve", lambda e: e.tensor_tensor(tmp.t[:], tmp.t[:], tmp2.t[:], ALU.subtract), reads=[tmp, tmp2], writes=[tmp])
        P.op("dve", lambda e: e.tensor_single_scalar(tmp2.t[:], tmp.t[:], 0.5, ALU.is_gt), reads=[tmp], writes=[tmp2])
        P.op("dve", lambda e: e.tensor_tensor(tmp.t[:], tmp.t[:], tmp2.t[:], ALU.subtract), reads=[tmp, tmp2], writes=[tmp])
        P.op("act", lambda e: e.activation(tab.t[:], tmp.t[:], AF.Sin, scale=scale_arg), reads=[tmp, invf], writes=[tab])
        P.dma("sp", c.rope_tab.t[slot], tab.t[:], reads=[tab], writes=[c.rope_tab], semkey=key)

    frac_sin(0.25, TWO_PI, 0, "tab")
    frac_sin(0.0, invf.t[0:32, 1:2], 1, "tab")
    P.release(m0)


def rstd_from_ss(P, out_ap, ss_ap, n, reads, writes, eng="dve"):
    P.op("act", lambda e: e.activation(out_ap, ss_ap, AF.Sqrt, bias=EPS, scale=1.0 / n), reads=reads, writes=writes)
    P.op(eng, lambda e: e.reciprocal(out_ap, out_ap), reads=writes, writes=writes)


def stage_h(c):
    P = c.P
    vec_ = c.vec
    m0 = P.mark()
    NB = 6
    DEPTH = 4
    xt = [P.sbuf(f"xt{i}", [128, D], F32) for i in range(NB)]
    junk = P.sbuf("hjunk", [128, D], F32)
    hb = [P.sbuf(f"hb{i}", [128, D], BF16) for i in range(2)]
    ht = [P.sbuf(f"ht{i}", [128, KC, 128], BF16) for i in range(2)]
    ss = [P.sbuf(f"hss{i}", [128, 1], F32) for i in range(2)]
    rs = [P.sbuf(f"hrs{i}", [128, 1], F32) for i in range(2)]
    c.hT_views = [P.view(f"hTv{i}") for i in range(S // 128)]
    gpre = vec_.t[:, V_GPRE:V_GPRE + KC]
    pst = c.pst
    nt = S // 128

    def load(i):
        x_ = xt[i % NB]
        rd = [c.xmid_views[i]] if c.x_in_buf is not None else []
        P.dma("sp", x_.t[:], c.x_in[i * 128:(i + 1) * 128, :], reads=rd, writes=[x_], semkey=K(x_))

    def stats(i):
        x_, s_, r_ = xt[i % NB], ss[i % 2], rs[i % 2]
        P.op("act", lambda e, s_=s_: e.memzero(s_.t[:]), writes=[s_])
        P.op("act", lambda e, x_=x_, s_=s_: e.activation(junk.t[:], x_.t[:], AF.Square, accum_out=s_.t[:]), reads=[x_, s_], writes=[junk, s_])
        rstd_from_ss(P, r_.t[:], s_.t[:], D, [s_], [r_])

    def apply(i):
        x_, r_, hb_, ht_ = xt[i % NB], rs[i % 2], hb[i % 2], ht[i % 2]
        P.op("act", lambda e, x_=x_, r_=r_, hb_=hb_: e.activation(hb_.t[:], x_.t[:], AF.Copy, scale=r_.t[:]), reads=[x_, r_], writes=[hb_])
        for kc in range(KC):
            P.op("pe", lambda e, kc=kc, hb_=hb_: e.transpose(pst.t[:, kc * 128:(kc + 1) * 128], hb_.t[:, kc * 128:(kc + 1) * 128], c.ident.t[:]),
                 reads=[hb_, c.ident], writes=[pst])
        P.op("dve", lambda e, ht_=ht_: e.tensor_tensor(ht_.t[:], pst.t[:].rearrange("p (k t) -> p k t", k=KC),
                                                      gpre.unsqueeze(2).to_broadcast([128, KC, 128]), ALU.mult),
             reads=[pst, vec_], writes=[ht_])
        P.dma("sp", c.hT.t[:, :, i * 128:(i + 1) * 128], ht_.t[:], reads=[ht_], writes=[c.hT_views[i]], semkey=K(ht_))

    for i in range(DEPTH):
        load(i)
    stats(0)
    for i in range(nt):
        if i + DEPTH < nt:
            load(i + DEPTH)
        if i + 1 < nt:
            stats(i + 1)
        apply(i)
    P.release(m0)


def load_w(c, dst, src_ap, key):
    c.P.dma("pool", dst.t[:] if not isinstance(dst, tuple) else dst[1], src_ap, writes=[dst if not isinstance(dst, tuple) else dst[0]], semkey=key)


def load_hT(c, buf, T):
    P = c.P
    rd = c.hT_views[T * 4:(T + 1) * 4]
    P.dma("sp", buf.t[:], c.hT.t[:, :, T * TT:(T + 1) * TT], reads=rd, writes=[buf], semkey=K(buf))


def proj_T(c, ps, w_ap_fn, hT_buf, wbuf, n_k=KC, M=128, ncols=TT, col0=0, extra_reads=()):
    P = c.P
    for kc in range(n_k):
        P.op("pe", lambda e, kc=kc: e.matmul(ps.t[0:M, 0:ncols], w_ap_fn(kc), hT_buf.t[:, kc, col0:col0 + ncols],
                                             start=(kc == 0), stop=(kc == n_k - 1)),
             reads=[wbuf, hT_buf] + list(extra_reads), writes=[ps])


def w_conv(c):
    P, l = c.P, c.l
    w = P.sbuf("wconv", [128, KC, 2048], BF16, top=True)
    for kc in range(KC):
        P.dma("pool", w.t[:, kc, :], c.w_in[l, kc * 128:(kc + 1) * 128, O_CX:O_CX + 2048], writes=[w], semkey="wconv")
    return w


def stage_conv(c):
    P = c.P
    vec_ = c.vec
    l = c.l
    m0 = P.mark()
    w = c.Wconv
    hT = [P.sbuf(f"chT{i}", [128, KC, TT], BF16) for i in range(2)]
    u = P.sbuf("cu", [128, 4, TT + 2], F32)
    cxs = P.sbuf("ccx", [128, TT], F32)
    y = P.sbuf("cy", [128, TT], F32)
    sg = P.sbuf("csg", [128, TT], F32)
    yo = [P.sbuf(f"cyo{i}", [128, 4, TT], BF16) for i in range(2)]
    P.op("dve", lambda e: e.memset(u.t[:], 0.0), writes=[u])
    ps = c.ps
    c.yc_views = [P.view(f"ycv{i}") for i in range(NT)]
    load_hT(c, hT[0], 0)
    for T in range(NT):
        h_ = hT[T % 2]
        if T + 1 < NT:
            load_hT(c, hT[(T + 1) % 2], T + 1)
        yo_ = yo[T % 2]
        for ch in range(4):
            par = (T * 4 + ch) % 2
            pcx, pcc, pgc, pcb = ps[0 + par], ps[2 + par], ps[4 + par], ps[6]
            for j, pp in ((0, pcx), (2, pcc), (3, pgc), (1, pcb)):
                proj_T(c, pp, lambda kc, j=j, ch=ch: w.t[:, kc, j * 512 + ch * 128: j * 512 + (ch + 1) * 128], h_, w)
            wc = lambda k, ch=ch: vec_.t[:, V_WCONV + ch * 3 + k: V_WCONV + ch * 3 + k + 1]
            P.op("act", lambda e, pcx=pcx: e.activation(cxs.t[:], pcx.t[:], AF.Copy), reads=[pcx], writes=[cxs])
            P.op("act", lambda e, pgc=pgc: e.activation(sg.t[:], pgc.t[:], AF.Silu), reads=[pgc], writes=[sg])
            P.op("dve", lambda e, ch=ch, pcc=pcc: e.tensor_tensor(u.t[:, ch, 2:TT + 2], pcc.t[:], cxs.t[:], ALU.mult), reads=[pcc, cxs], writes=[u])
            P.op("dve", lambda e, ch=ch, wc=wc: e.tensor_scalar(y.t[:], u.t[:, ch, 2:TT + 2], wc(2), None, ALU.mult), reads=[u, vec_], writes=[y])
            P.op("dve", lambda e, ch=ch, wc=wc: e.scalar_tensor_tensor(y.t[:], u.t[:, ch, 1:TT + 1], wc(1), y.t[:], ALU.mult, ALU.add), reads=[u, y, vec_], writes=[y])
            P.op("dve", lambda e, ch=ch, wc=wc: e.scalar_tensor_tensor(y.t[:], u.t[:, ch, 0:TT], wc(0), y.t[:], ALU.mult, ALU.add), reads=[u, y, vec_], writes=[y])
            P.op("dve", lambda e, ch=ch, pcb=pcb: e.scalar_tensor_tensor(y.t[:], y.t[:], vec_.t[:, V_BCONV + ch:V_BCONV + ch + 1], pcb.t[:], ALU.add, ALU.mult),
                 reads=[y, pcb, vec_], writes=[y])
            P.op("dve", lambda e, ch=ch, yo_=yo_: e.tensor_tensor(yo_.t[:, ch, :], y.t[:], sg.t[:], ALU.mult), reads=[y, sg], writes=[yo_])
            P.op("dve", lambda e, ch=ch: e.tensor_copy(u.t[:, ch, 0:2], u.t[:, ch, TT:TT + 2]), reads=[u], writes=[u])
        P.dma("pool", c.yT[2].t[:, :, T * TT:(T + 1) * TT], yo_.t[:], reads=[yo_], writes=[c.yc_views[T]], semkey=K(yo_))
    P.release(m0)


def w_final(c):
    P, l = c.P, c.l
    wg = P.sbuf("wg", [128, KC, 4 * D], BF16, top=True)
    for kc in range(KC):
        P.dma("pool", wg.t[:, kc, :], c.w_gate[l, kc * 128:(kc + 1) * 128, :], writes=[wg], semkey="wg")
    wp = P.sbuf("wp", [128, 4, 4, D], BF16, top=True)
    for b in range(4):
        P.dma("pool", wp.t[:, b, :, :], c.w_p[l, b].rearrange("(k p) n -> p k n", p=128), writes=[wp], semkey="wp")
    wo = P.sbuf("wo", [128, KC, D], BF16, top=True)
    P.dma("pool", wo.t[:], c.w_out[l].rearrange("(k p) n -> p k n", p=128), writes=[wo], semkey="wo")
    gpost = P.sbuf("gpost", [128, D], F32, top=True)
    P.dma("sp", gpost.t[:], c.rows[l, R_GPOST:R_GPOST + 1, :].to_broadcast([128, D]), writes=[gpost], semkey="gpost")
    return wg, wp, wo, gpost


def stage_final(c):
    P = c.P
    vec_ = c.vec
    l = c.l
    m0 = P.mark()
    wg, wp, wo, gpost = c.Wfinal
    hbuf = [P.sbuf(f"fhT{i}", [128, KC, TT], BF16) for i in range(2)]
    ytb = [P.sbuf(f"fyt{b}", [128, 4, TT], BF16) for b in range(4)]
    sig = [P.sbuf(f"fsig{i}", [128, TT], F32) for i in range(2)]
    acc = P.sbuf("facc", [128, TT], F32)
    tmp = [P.sbuf(f"ftmp{i}", [128, TT], F32) for i in range(2)]
    mTb = [P.sbuf(f"fmT{i}", [128, KC, TT], BF16) for i in range(2)]
    xt = [P.sbuf(f"fxt{i}", [128, D], F32) for i in range(2)]
    ot = [P.sbuf(f"fot{i}", [128, D], F32) for i in range(2)]
    junk = P.sbuf("fjunk", [128, TT], F32)
    ss2 = [P.sbuf(f"fss{i}", [128, 2], F32) for i in range(2)]
    rs = [P.sbuf(f"frs{i}", [128, 1], F32) for i in range(2)]
    ps = c.ps
    yviews = [c.ya_views, c.yb_views, c.yc_views, c.yd_views]

    def gate_block(T, cch, h_, mT):
        for b in range(4):
            pg, pp, sg_ = ps[b % 2], ps[2 + b % 2], sig[b % 2]
            proj_T(c, pg, lambda kc, b=b, cch=cch: wg.t[:, kc, b * D + cch * 128: b * D + (cch + 1) * 128], h_, wg)
            bcol = vec_.t[:, V_BGATE + b * 8 + cch: V_BGATE + b * 8 + cch + 1]
            P.op("act", lambda e, pg=pg, sg_=sg_, bcol=bcol: e.activation(sg_.t[:], pg.t[:], AF.Sigmoid, bias=bcol), reads=[pg, vec_], writes=[sg_])
            for kc in range(4):
                P.op("pe", lambda e, kc=kc, b=b, cch=cch, pp=pp: e.matmul(pp.t[:], wp.t[:, b, kc, cch * 128:(cch + 1) * 128], ytb[b].t[:, kc, :],
                                                                   start=(kc == 0), stop=(kc == 3)), reads=[wp, ytb[b]], writes=[pp])
            if b == 0:
                P.op("dve", lambda e, pp=pp, sg_=sg_: e.tensor_tensor(acc.t[:], pp.t[:], sg_.t[:], ALU.mult), reads=[pp, sg_], writes=[acc])
            else:
                t_ = tmp[b % 2]
                P.op("dve", lambda e, pp=pp, sg_=sg_, t_=t_: e.tensor_tensor(t_.t[:], pp.t[:], sg_.t[:], ALU.mult), reads=[pp, sg_], writes=[t_])
                if b < 3:
                    P.op("pool", lambda e, t_=t_: e.tensor_tensor(acc.t[:], acc.t[:], t_.t[:], ALU.add), reads=[acc, t_], writes=[acc])
                else:
                    P.op("pool", lambda e, t_=t_, cch=cch, mT=mT: e.tensor_tensor(mT.t[:, cch, :], acc.t[:], t_.t[:], ALU.add), reads=[acc, t_], writes=[mT])

    def out_tile(T, t4, mT):
        i = T * 4 + t4
        x_, o_, s_, r_ = xt[i % 2], ot[i % 2], ss2[i % 2], rs[i % 2]
        rd = [c.xmid_views[i]] if c.x_in_buf is not None else []
        P.dma("sp", x_.t[:], c.x_in[i * 128:(i + 1) * 128, :], reads=rd, writes=[x_], semkey=K(x_))
        P.op("dve", lambda e, s_=s_: e.memset(s_.t[:], 0.0), writes=[s_])
        for half in range(2):
            po = ps[4 + half]
            for kc in range(KC):
                P.op("pe", lambda e, kc=kc, half=half, po=po, t4=t4, mT=mT: e.matmul(po.t[:], mT.t[:, kc, t4 * 128:(t4 + 1) * 128], wo.t[:, kc, half * 512:(half + 1) * 512],
                                                                       start=(kc == 0), stop=(kc == KC - 1)), reads=[mT, wo], writes=[po])
            P.op("act", lambda e, po=po, s_=s_, half=half: e.activation(junk.t[:], po.t[:], AF.Square, accum_out=s_.t[:, half:half + 1]),
                 reads=[po, s_], writes=[junk, s_])
        P.op("dve", lambda e, s_=s_: e.tensor_tensor(s_.t[:, 0:1], s_.t[:, 0:1], s_.t[:, 1:2], ALU.add), reads=[s_], writes=[s_])
        rstd_from_ss(P, r_.t[:], s_.t[:, 0:1], D, [s_], [r_])
        for half in range(2):
            po = ps[4 + half]
            sl = slice(half * 512, (half + 1) * 512)
            P.op("dve", lambda e, po=po, sl=sl, r_=r_, o_=o_: e.scalar_tensor_tensor(o_.t[:, sl], po.t[:], r_.t[:], gpost.t[:, sl], ALU.mult, ALU.mult),
                 reads=[po, r_, gpost], writes=[o_])
        P.op("pool", lambda e, o_=o_, x_=x_: e.tensor_tensor(o_.t[:], o_.t[:], x_.t[:], ALU.add), reads=[o_, x_], writes=[o_])
        wr = [c.xmid_views[i]] if c.x_out_buf is not None else []
        P.dma("pool", c.x_out[i * 128:(i + 1) * 128, :], o_.t[:], reads=[o_], writes=wr, semkey=K(o_))

    load_hT(c, hbuf[0], 0)
    for T in range(NT + 1):
        if T < NT:
            h_ = hbuf[T % 2]
            for b in range(4):
                P.dma("sp", ytb[b].t[:], c.yT_src[b][:, :, T * TT:(T + 1) * TT], reads=[yviews[b][T]], writes=[ytb[b]], semkey=K(ytb[b]))
            if T + 1 < NT:
                load_hT(c, hbuf[(T + 1) % 2], T + 1)
        for cch in range(KC):
            if T < NT:
                gate_block(T, cch, h_, mTb[T % 2])
            if T >= 1 and cch % 2 == 1:
                out_tile(T - 1, cch // 2, mTb[(T - 1) % 2])
    P.release(m0)


def stage_sgu(c):
    P = c.P
    vec_ = c.vec
    l = c.l
    m0 = P.mark()
    w = P.sbuf("wsgu", [128, KC, 1536], BF16)
    for kc in range(KC):
        P.dma("pool", w.t[:, kc, 0:1024], c.w_in[l, kc * 128:(kc + 1) * 128, O_DUV:O_DUV + 1024], writes=[w], semkey="wsgu")
        P.dma("pool", w.t[:, kc, 1024:1536], c.w_in[l, kc * 128:(kc + 1) * 128, O_GD:O_GD + 512], writes=[w], semkey="wsgu")
    wsf = P.sbuf("wsf", [128, 4, 128], F32)
    P.dma("sp", wsf.t[:], c.w_sT[l].rearrange("g s t -> s g t"), writes=[wsf], semkey="wsf")
    wsm = P.sbuf("wsm", [128, 4, 128], BF16)
    P.op("dve", lambda e: e.tensor_tensor(wsm.t[:], wsf.t[:], c.cf.t[:, C_TRIL128:C_TRIL128 + 1, :].to_broadcast([128, 4, 128]), ALU.mult),
         reads=[wsf, c.cf], writes=[wsm])
    bsb = P.sbuf("bsb", [128, 512], F32)
    P.dma("sp", bsb.t[:], c.rows[l, R_BS:R_BS + 1, 0:512].to_broadcast([128, 512]), writes=[bsb], semkey="bsb")
    if c.after_w is not None:
        c.after_w()
    hT = [P.sbuf(f"dhT{i}", [128, KC, TT], BF16) for i in range(2)]
    ug = P.sbuf("dug", [128, 4, TT], F32)
    sgd = P.sbuf("dsgd", [128, TT], F32)
    vg = [P.sbuf(f"dvg{i}", [128, 512], F32) for i in range(2)]
    junk = P.sbuf("djunk", [128, 512], F32)
    st = [P.sbuf(f"dst{i}", [128, 4], F32) for i in range(2)]
    vn = [P.sbuf(f"dvn{i}", [128, 512], BF16) for i in range(8)]
    mx = P.sbuf("dmx", [128, 4, 128], F32)
    yo = [P.sbuf(f"dyo{i}", [128, 4, TT], BF16) for i in range(2)]
    ps = c.ps
    c.yd_views = [P.view(f"ydv{i}") for i in range(NT)]
    load_hT(c, hT[0], 0)

    def vpart(T):
        h_ = hT[T % 2]
        for t4 in range(4):
            pv, vg_, st_, vn_ = ps[2 + t4 % 2], vg[t4 % 2], st[t4 % 2], vn[(T % 2) * 4 + t4]
            for kc in range(KC):
                P.op("pe", lambda e, kc=kc, t4=t4, pv=pv, h_=h_: e.matmul(pv.t[:], h_.t[:, kc, t4 * 128:(t4 + 1) * 128], w.t[:, kc, 512:1024],
                                                            start=(kc == 0), stop=(kc == KC - 1)), reads=[h_, w], writes=[pv])
            P.op("act", lambda e, st_=st_: e.memzero(st_.t[:]), writes=[st_])
            P.op("act", lambda e, pv=pv, vg_=vg_, st_=st_: e.activation(vg_.t[:], pv.t[:], AF.Gelu_apprx_tanh, accum_out=st_.t[:, 0:1]),
                 reads=[pv, st_], writes=[vg_, st_])
            P.op("act", lambda e, vg_=vg_, st_=st_: e.activation(junk.t[:], vg_.t[:], AF.Square, accum_out=st_.t[:, 1:2]),
                 reads=[vg_, st_], writes=[junk, st_])
            P.op("dve", lambda e, st_=st_: e.tensor_scalar(st_.t[:, 0:2], st_.t[:, 0:2], 1.0 / 512, None, ALU.mult), reads=[st_], writes=[st_])
            P.op("dve", lambda e, st_=st_: e.tensor_tensor(st_.t[:, 2:3], st_.t[:, 0:1], st_.t[:, 0:1], ALU.mult), reads=[st_], writes=[st_])
            P.op("dve", lambda e, st_=st_: e.tensor_tensor(st_.t[:, 3:4], st_.t[:, 1:2], st_.t[:, 2:3], ALU.subtract), reads=[st_], writes=[st_])
            P.op("act", lambda e, st_=st_: e.activation(st_.t[:, 3:4], st_.t[:, 3:4], AF.Sqrt, bias=EPS, scale=1.0), reads=[st_], writes=[st_])
            P.op("dve", lambda e, st_=st_: e.reciprocal(st_.t[:, 3:4], st_.t[:, 3:4]), reads=[st_], writes=[st_])
            P.op("dve", lambda e, st_=st_, vg_=vg_, vn_=vn_: e.tensor_scalar(vn_.t[:], vg_.t[:], st_.t[:, 0:1], st_.t[:, 3:4], ALU.subtract, ALU.mult),
                 reads=[vg_, st_], writes=[vn_])

    def upart(T, yo_):
        h_ = hT[T % 2]
        for ch in range(4):
            pu, pg = ps[0], ps[1]
            if ch % 2 == 1:
                pu, pg = ps[6], ps[4]
            proj_T(c, pu, lambda kc, ch=ch: w.t[:, kc, ch * 128:(ch + 1) * 128], h_, w)
            proj_T(c, pg, lambda kc, ch=ch: w.t[:, kc, 1024 + ch * 128:1024 + (ch + 1) * 128], h_, w)
            P.op("act", lambda e, ch=ch, pu=pu: e.activation(ug.t[:, ch, :], pu.t[:], AF.Gelu_apprx_tanh), reads=[pu], writes=[ug])
            P.op("act", lambda e, pg=pg: e.activation(sgd.t[:], pg.t[:], AF.Silu), reads=[pg], writes=[sgd])
            P.op("dve", lambda e, ch=ch: e.tensor_tensor(ug.t[:, ch, :], ug.t[:, ch, :], sgd.t[:], ALU.mult), reads=[ug, sgd], writes=[ug])

    def mpart(T, yo_):
        for g in range(4):
            pm = ps[4 + g % 2]
            for t4 in range(4):
                P.op("pe", lambda e, g=g, t4=t4, pm=pm, T=T: e.matmul(pm.t[:, t4 * 128:(t4 + 1) * 128], vn[(T % 2) * 4 + t4].t[:, g * 128:(g + 1) * 128], wsm.t[:, g, :],
                                                          start=True, stop=True), reads=[vn[(T % 2) * 4 + t4], wsm], writes=[pm])
            P.op("dve", lambda e, g=g, pm=pm: e.scalar_tensor_tensor(mx.t[:], pm.t[:].rearrange("p (a t) -> p a t", a=4),
                                                                   vec_.t[:, V_GSV + g:V_GSV + g + 1],
                                                                   bsb.t[:, g * 128:(g + 1) * 128].unsqueeze(1).to_broadcast([128, 4, 128]), ALU.mult, ALU.add),
                 reads=[pm, vec_, bsb], writes=[mx])
            P.op("dve", lambda e, g=g, yo_=yo_: e.tensor_tensor(yo_.t[:, g, :], mx.t[:].rearrange("p a t -> p (a t)"), ug.t[:, g, :], ALU.mult),
                 reads=[mx, ug], writes=[yo_])
        P.dma("pool", c.yT[3].t[:, :, T * TT:(T + 1) * TT], yo_.t[:], reads=[yo_], writes=[c.yd_views[T]], semkey=K(yo_))

    vpart(0)
    for T in range(NT):
        if T + 1 < NT:
            load_hT(c, hT[(T + 1) % 2], T + 1)
        yo_ = yo[T % 2]
        upart(T, yo_)
        if T + 1 < NT:
            vpart(T + 1)
        mpart(T, yo_)
    P.release(m0)


def w_attn(c):
    P, l = c.P, c.l
    vec_ = c.vec
    c.attn_stg_mark = P.mark()
    wA = P.sbuf("wA", [128, KC, 896], BF16, top=True)
    for kc in range(KC):
        P.dma("pool", wA.t[:, kc, 0:384], c.w_in[l, kc * 128:(kc + 1) * 128, O_CQ:O_CQ + 384], writes=[wA], semkey="wA")
        P.dma("pool", wA.t[:, kc, 384:896], c.w_in[l, kc * 128:(kc + 1) * 128, O_GA:O_GA + 512], writes=[wA], semkey="wA")
    wkr = P.sbuf("wkr", [128, KC, 2, 96], BF16, top=True)
    P.dma("pool", wkr.t[:], c.w_kr[l].rearrange("(k p) a n -> p k a n", p=128), writes=[wkr], semkey="wkr")
    wuq = P.sbuf("wuq", [128, 2, 768], BF16, top=True)
    wuqs = P.sbuf("wuqs", [128, 2, 768], BF16, top=True)
    wk = P.sbuf("wk", [128, 8, 64], BF16, top=True)
    wv = P.sbuf("wv", [128, 8, 64], BF16, top=True)
    stg = P.sbuf("astg", [128, 2, 768], F32)
    stg2 = P.sbuf("astg2", [128, 8, 128], F32)
    for src, dst in ((c.w_uq, wuq), (c.w_uq_sw, wuqs)):
        P.dma("sp", stg.t[:], src[l].rearrange("(k p) n -> p k n", p=128), writes=[stg], semkey="astg")
        for j in range(2):
            P.op("dve", lambda e, j=j, dst=dst: e.tensor_scalar(dst.t[:, j, :], stg.t[:, j, :], vec_.t[:, V_GCQ + j:V_GCQ + j + 1], None, ALU.mult),
                 reads=[stg, vec_], writes=[dst])
    P.dma("sp", stg2.t[:], c.w_ukv[l].rearrange("k (h x) -> k h x", x=128), writes=[stg2], semkey="astg2")
    gk = vec_.t[:, V_GCKV:V_GCKV + 1]
    P.op("dve", lambda e: e.tensor_scalar(wk.t[:], stg2.t[:, :, 0:64], gk, None, ALU.mult), reads=[stg2, vec_], writes=[wk])
    P.op("dve", lambda e: e.tensor_scalar(wv.t[:], stg2.t[:, :, 64:128], gk, None, ALU.mult), reads=[stg2, vec_], writes=[wv])
    return wA, wkr, wuq, wuqs, wk, wv


def stage_attn(c):
    P = c.P
    vec_ = c.vec
    l = c.l
    m0 = P.mark()
    ps = c.ps
    cf = c.cf
    wA, wkr, wuq, wuqs, wk, wv = c.Wattn
    KT = P.sbuf("KT", [128, 8, S], BF16)
    VA = P.sbuf("VA", [128, S // 128, 8, 72], BF16)
    P.op("pool", lambda e: e.memset(VA.t[:, :, :, 64:65], 1.0), writes=[VA])
    h_ = P.sbuf("ahT", [128, KC, TT], BF16)
    cqb = P.sbuf("cqb", [128, 2, TT], BF16)
    ckvb = P.sbuf("ckvb", [128, TT], BF16)
    sqf = [P.sbuf(f"sqf{i}", [128, TT], F32) for i in range(2)]
    rq_b = P.sbuf("rq_b", [128, TT], F32)
    rkv_b = P.sbuf("rkv_b", [128, TT], F32)
    rkv_tok = P.sbuf("rkv_tok", [128, 4], F32)
    C2t = P.sbuf("C2t", [128, TT], F32)
    S2t = P.sbuf("S2t", [128, TT], F32)
    C2r = P.sbuf("C2r", [128, TT], F32)
    S2r = P.sbuf("S2r", [128, TT], F32)
    kr = P.sbuf("kr", [128, TT], BF16)
    t1 = P.sbuf("at1", [128, TT], F32)
    t2 = P.sbuf("at2", [128, TT], F32)
    t1b = P.sbuf("at1b", [128, TT], F32)
    t2b = P.sbuf("at2b", [128, TT], F32)
    nb = [0]
    QT = P.sbuf("QT", [128, 8, TT], BF16)
    PT = [P.sbuf(f"PT{i}", [128, TT], BF16) for i in range(3)]
    Osb = [P.sbuf(f"Osb{i}", [128, TT], F32) for i in range(2)]
    rden = P.sbuf("rden", [128, TT], F32)
    yst = [P.sbuf(f"yst{i}", [128, TT], F32) for i in range(2)]
    sga4 = P.sbuf("sga4", [128, 4, TT], BF16)
    yo = [P.sbuf(f"ayo{i}", [128, 4, TT], BF16) for i in range(2)]
    c.ya_views = [P.view(f"yav{i}") for i in range(NT)]
    scale = float(96 ** -0.5)
    ones_f = cf.t[:, C_ONES, :]
    R = slice(64, 96)
    nS = 0
    for T in range(NT):
        cols = slice(T * TT, (T + 1) * TT)
        if T == 0:
            load_hT(c, h_, 0)
        P.dma("sp", C2t.t[R, :], c.rope_tab.t[0][:, cols], reads=[c.rope_tab], writes=[C2t], semkey="C2t")
        P.dma("sp", S2t.t[R, :], c.rope_tab.t[1][:, cols], reads=[c.rope_tab], writes=[S2t], semkey="S2t")

        def nxt():
            b_ = ps[nb[0] % 7]
            nb[0] += 1
            return b_

        pcq = [nxt(), nxt()]
        for j in range(2):
            proj_T(c, pcq[j], lambda kc, j=j: wA.t[:, kc, j * 128:(j + 1) * 128], h_, wA)
            P.op("act", lambda e, j=j, pp=pcq[j]: e.activation(cqb.t[:, j, :], pp.t[:], AF.Copy), reads=[pcq[j]], writes=[cqb])
            P.op("act", lambda e, j=j, pp=pcq[j]: e.activation(sqf[j].t[:], pp.t[:], AF.Square), reads=[pcq[j]], writes=[sqf[j]])
        pkv = nxt()
        proj_T(c, pkv, lambda kc: wA.t[:, kc, 256:384], h_, wA)
        pss = nxt()
        for j in range(2):
            P.op("pe", lambda e, j=j, pss=pss: e.matmul(pss.t[:], ones_f, sqf[j].t[:], start=(j == 0), stop=(j == 1)), reads=[cf, sqf[j]], writes=[pss])
        rstd_from_ss(P, rq_b.t[:], pss.t[:], 256, [pss], [rq_b])
        P.op("act", lambda e, pkv=pkv: e.activation(ckvb.t[:], pkv.t[:], AF.Copy), reads=[pkv], writes=[ckvb])
        P.op("act", lambda e, pkv=pkv: e.activation(sqf[0].t[:], pkv.t[:], AF.Square), reads=[pkv], writes=[sqf[0]])
        pkr = [nxt(), nxt()]
        for a in range(2):
            proj_T(c, pkr[a], lambda kc, a=a: wkr.t[:, kc, a, :], h_, wkr, M=96)
        P.op("dve", lambda e, pp=pkr[0]: e.tensor_tensor(t1.t[R, :], pp.t[R, :], C2t.t[R, :], ALU.mult), reads=[pkr[0], C2t], writes=[t1])
        P.op("dve", lambda e, pp=pkr[1]: e.tensor_tensor(t2.t[R, :], pp.t[R, :], S2t.t[R, :], ALU.mult), reads=[pkr[1], S2t], writes=[t2])
        P.op("pool", lambda e: e.tensor_tensor(kr.t[R, :], t1.t[R, :], t2.t[R, :], ALU.add), reads=[t1, t2], writes=[kr])
        P.op("pool", lambda e, cols=cols: e.tensor_copy(KT.t[R, :, cols], kr.t[R, :].unsqueeze(1).to_broadcast([32, 8, TT])), reads=[kr], writes=[KT])
        for ch in range(4):
            pga = nxt()
            proj_T(c, pga, lambda kc, ch=ch: wA.t[:, kc, 384 + ch * 128:384 + (ch + 1) * 128], h_, wA)
            P.op("act", lambda e, ch=ch, pga=pga: e.activation(sga4.t[:, ch, :], pga.t[:], AF.Silu), reads=[pga], writes=[sga4])
        if T + 1 < NT:
            load_hT(c, h_, T + 1)
        pss2 = nxt()
        P.op("pe", lambda e, pss2=pss2: e.matmul(pss2.t[:], ones_f, sqf[0].t[:], start=True, stop=True), reads=[cf, sqf[0]], writes=[pss2])
        rstd_from_ss(P, rkv_b.t[:], pss2.t[:], 128, [pss2], [rkv_b])
        pst_ = nxt()
        for t4 in range(4):
            P.op("pe", lambda e, t4=t4, pst_=pst_: e.matmul(pst_.t[:, t4:t4 + 1], sqf[0].t[:, t4 * 128:(t4 + 1) * 128], cf.t[:, C_ONES, 0:1], start=True, stop=True),
                 reads=[cf, sqf[0]], writes=[pst_])
        rstd_from_ss(P, rkv_tok.t[:], pst_.t[:, 0:4], 128, [pst_], [rkv_tok])
        P.op("pool", lambda e: e.tensor_tensor(C2r.t[R, :], C2t.t[R, :], rq_b.t[R, :], ALU.mult), reads=[C2t, rq_b], writes=[C2r])
        P.op("pool", lambda e: e.tensor_tensor(S2r.t[R, :], S2t.t[R, :], rq_b.t[R, :], ALU.mult), reads=[S2t, rq_b], writes=[S2r])
        for h in range(8):
            pk = nxt()
            P.op("pe", lambda e, h=h, pk=pk: e.matmul(pk.t[0:64, :], wk.t[:, h, :], ckvb.t[:], start=True, stop=True), reads=[wk, ckvb], writes=[pk])
            P.op("dve", lambda e, h=h, pk=pk, cols=cols: e.tensor_tensor(KT.t[0:64, h, cols], pk.t[0:64, :], rkv_b.t[0:64, :], ALU.mult), reads=[pk, rkv_b], writes=[KT])
        for t4 in range(4):
            pv = nxt()
            P.op("pe", lambda e, t4=t4, pv=pv: e.matmul(pv.t[:], ckvb.t[:, t4 * 128:(t4 + 1) * 128], wv.t[:].rearrange("p h x -> p (h x)"), start=True, stop=True),
                 reads=[wv, ckvb], writes=[pv])
            P.op("dve", lambda e, t4=t4, pv=pv, T=T: e.tensor_scalar(VA.t[:, T * 4 + t4, :, 0:64], pv.t[:].rearrange("p (h x) -> p h x", h=8),
                                                             rkv_tok.t[:, t4:t4 + 1], None, ALU.mult), reads=[pv, rkv_tok], writes=[VA])
        tq = [(t1, t2), (t1b, t2b)]
        for h in range(8):
            pqa, pqb = nxt(), nxt()
            for a, (wq, pq_) in enumerate(((wuq, pqa), (wuqs, pqb))):
                for j in range(2):
                    P.op("pe", lambda e, j=j, h=h, wq=wq, pq_=pq_: e.matmul(pq_.t[0:96, :], wq.t[:, j, h * 96:(h + 1) * 96], cqb.t[:, j, :], start=(j == 0), stop=(j == 1)),
                         reads=[wq, cqb], writes=[pq_])
            ta, tb = tq[h % 2]
            P.op("dve", lambda e, h=h, pqa=pqa: e.tensor_tensor(QT.t[0:64, h, :], pqa.t[0:64, :], rq_b.t[0:64, :], ALU.mult), reads=[pqa, rq_b], writes=[QT])
            P.op("dve", lambda e, pqa=pqa, ta=ta: e.tensor_tensor(ta.t[R, :], pqa.t[R, :], C2r.t[R, :], ALU.mult), reads=[pqa, C2r], writes=[ta])
            P.op("dve", lambda e, pqb=pqb, tb=tb: e.tensor_tensor(tb.t[R, :], pqb.t[R, :], S2r.t[R, :], ALU.mult), reads=[pqb, S2r], writes=[tb])
            P.op("pool", lambda e, h=h, ta=ta, tb=tb: e.tensor_tensor(QT.t[R, h, :], ta.t[R, :], tb.t[R, :], ALU.add), reads=[ta, tb], writes=[QT])
        yo_ = yo[T % 2]
        blocks = []
        for h in range(8):
            nkb = 4 * T + 4
            for kb in range(nkb):
                parts = []
                if kb < 4 * T:
                    parts.append((128, 0, TT))
                else:
                    j = kb - 4 * T
                    parts.append((64, 2 * j * 64, (2 * j + 1) * 64))
                    if (2 * j + 1) * 64 < TT:
                        parts.append((128, (2 * j + 1) * 64, TT))
                blocks.append((h, kb, parts, kb == nkb - 1))
        nblk = len(blocks)
        LOOK = 2
        deferred = []

        def emit_S(i):
            h, kb, parts, _ = blocks[i]
            pS, pt = ps[2 + (nS + i) % 3], PT[(nS + i) % 3]
            for (rows, c0, c1) in parts:
                P.op("pe", lambda e, h=h, kb=kb, rows=rows, c0=c0, c1=c1, pS=pS: e.matmul(pS.t[0:rows, c0:c1], KT.t[0:96, h, kb * 128:kb * 128 + rows], QT.t[0:96, h, c0:c1],
                                                                                     start=True, stop=True), reads=[KT, QT], writes=[pS])
            for (rows, c0, c1) in parts:
                P.op("act", lambda e, rows=rows, c0=c0, c1=c1, pS=pS, pt=pt: e.activation(pt.t[0:rows, c0:c1], pS.t[0:rows, c0:c1], AF.Exp, scale=scale),
                     reads=[pS], writes=[pt])

        def emit_PV(i):
            h, kb, parts, lastkb = blocks[i]
            pt = PT[(nS + i) % 3]
            pO = ps[5 + h % 2]
            for pi, (rows, c0, c1) in enumerate(parts):
                last = lastkb and (pi == len(parts) - 1)
                P.op("pe", lambda e, h=h, kb=kb, rows=rows, c0=c0, c1=c1, pt=pt, pO=pO, last=last: e.matmul(pO.t[0:65, c0:c1], VA.t[0:rows, kb, h, 0:65], pt.t[0:rows, c0:c1],
                                                                                                start=(kb == 0), stop=last), reads=[VA, pt], writes=[pO])
            if lastkb:
                ob = Osb[h % 2]
                P.op("act", lambda e, ob=ob, pO=pO: e.activation(ob.t[0:65, :], pO.t[0:65, :], AF.Copy), reads=[pO], writes=[ob])
                deferred.append((i + LOOK, h))

        def emit_epilogue(h):
            ob = Osb[h % 2]
            P.op("pe", lambda e, ob=ob: e.matmul(ps[0].t[0:64, :], cf.t[0:65, C_SELDEN, 0:64], ob.t[0:65, :], start=True, stop=True), reads=[cf, ob], writes=[ps[0]])
            P.op("dve", lambda e: e.reciprocal(rden.t[0:64, :], ps[0].t[0:64, :]), reads=[ps[0]], writes=[rden])
            ys = yst[(h // 2) % 2]
            pb = (h % 2) * 64
            P.op("dve", lambda e, ob=ob, ys=ys, pb=pb: e.tensor_tensor(ys.t[pb:pb + 64, :], ob.t[0:64, :], rden.t[0:64, :], ALU.mult), reads=[ob, rden], writes=[ys])
            if h % 2 == 1:
                ch = h // 2
                P.op("pool", lambda e, ch=ch, ys=ys, yo_=yo_: e.tensor_tensor(yo_.t[:, ch, :], ys.t[:], sga4.t[:, ch, :], ALU.mult), reads=[ys, sga4], writes=[yo_])

        for i in range(nblk + LOOK):
            if i < nblk:
                emit_S(i)
            j = i - LOOK
            if j >= 0:
                emit_PV(j)
            while deferred and deferred[0][0] <= j:
                emit_epilogue(deferred.pop(0)[1])
        while deferred:
            emit_epilogue(deferred.pop(0)[1])
        nS += nblk
        P.dma("pool", c.yT[0].t[:, :, cols], yo_.t[:], reads=[yo_], writes=[c.ya_views[T]], semkey=K(yo_))
    P.release(m0)


def stage_mlstm(c):
    P = c.P
    vec_ = c.vec
    l = c.l
    m0 = P.mark()
    ps = c.ps
    cf = c.cf
    NB_ = 2056
    wB = P.sbuf("wB", [128, KC, NB_], BF16)
    wBa = P.view("wBa")
    for kc in range(KC):
        P.dma("pool", wB.t[:, kc, 0:1032], c.w_in[l, kc * 128:(kc + 1) * 128, O_MQ:O_MQ + 1032], writes=[wBa], semkey="wBa")
    for kc in range(KC):
        P.dma("pool", wB.t[:, kc, 1032:NB_], c.w_in[l, kc * 128:(kc + 1) * 128, O_MQ + 1032:O_MQ + NB_], writes=[wB], semkey="wB")
    if c.after_w is not None:
        c.after_w()
    gmh = P.sbuf("gmh", [128, 512], F32)
    P.dma("sp", gmh.t[:], c.rows[l, R_GMH:R_GMH + 1, 0:512].to_broadcast([128, 512]), writes=[gmh], semkey="gmh")
    fbb = P.sbuf("fbb", [128, 4], F32)
    P.dma("sp", fbb.t[:], c.rows[l, R_FB:R_FB + 1, 0:4].to_broadcast([128, 4]), writes=[fbb], semkey="fbb")
    hTb = [P.sbuf(f"bhT{i}", [128, KC, TT], BF16) for i in range(2)]
    qT = P.sbuf("bqT", [64, 4, TT], BF16)
    qz = [P.sbuf(f"bqz{i}", [64, 4, TT], BF16) for i in range(2)]
    kT = P.sbuf("bkT", [64, 4, TT], BF16)
    ktok = P.sbuf("bktok", [128, 4, 4, 64], BF16)
    vtmp = P.sbuf("bvtmp", [128, 4, 512], F32)
    graw = P.sbuf("bgraw", [128, 4, 8], F32)
    fp = P.sbuf("bfp", [128, 4, 4], F32)
    l1 = P.sbuf("bl1", [128, 4, 4], F32)
    eb = P.sbuf("beb", [128, 4, 4], F32)
    ui = P.sbuf("bui", [128, 4, 4], F32)
    uu = P.sbuf("buu", [128, 4, 4], F32)
    egr = [P.sbuf(f"begr{i}", [64, 2, 4, 4], F32) for i in range(2)]
    vp = P.sbuf("bvp", [128, 4, 4, 136], BF16)
    Usb = P.sbuf("bUsb", [64, 8, 4, 129], F32)
    Z = P.sbuf("bZ", [64, 4, 129], F32)
    Cb = [P.sbuf(f"bCb{i}", [64, 4, 136], BF16) for i in range(8)]
    STm = [P.sbuf(f"bSTm{i}", [128, 4, 128], BF16) for i in range(4)]
    dn = P.sbuf("bdn", [128, 4], F32)
    rr = P.sbuf("brr", [128, 4], F32)
    hh = P.sbuf("bhh", [128, 4, 128], F32)
    sq = P.sbuf("bsq", [128, 4, 128], F32)
    ssm = P.sbuf("bssm", [128, 4], F32)
    so4 = [P.sbuf(f"bso{i}", [128, 512], F32) for i in range(4)]
    sg = P.sbuf("bsg", [128, 512], F32)
    ybt = [P.sbuf(f"bybt{i}", [128, 512], BF16) for i in range(2)]
    yo = [P.sbuf(f"byo{i}", [128, 4, TT], BF16) for i in range(2)]
    c.yb_views = [P.view(f"ybv{i}") for i in range(NT)]
    for i in range(2):
        P.op("pool", lambda e, i=i: e.memset(qz[i].t[:], 0.0), writes=[qz[i]])
    P.op("pool", lambda e: e.memset(vp.t[:], 0.0), writes=[vp])
    for i in range(8):
        P.op("pool", lambda e, i=i: e.memset(Cb[i].t[:], 0.0), writes=[Cb[i]])
    load_hT(c, hTb[0], 0)
    for T in range(NT):
        cols = slice(T * TT, (T + 1) * TT)
        h_ = hTb[T % 2]
        if T + 1 < NT:
            load_hT(c, hTb[(T + 1) % 2], T + 1)
        eg_cur, eg_prev = egr[T % 2], egr[(T + 1) % 2]
        for h in range(4):
            pq = ps[h % 2]
            proj_T(c, pq, lambda kc, h=h: wB.t[:, kc, h * 64:(h + 1) * 64], h_, wBa, M=64)
            P.op("act", lambda e, h=h, pq=pq: e.activation(qT.t[0:64, h, :], pq.t[0:64, :], AF.Copy, scale=0.125), reads=[pq], writes=[qT])
            for par in range(2):
                P.op("pool", lambda e, h=h, par=par: e.tensor_copy(qz[par].t[0:64, h, :].rearrange("p (a b x) -> p a b x", a=4, b=2)[:, :, par, :],
                                                                  qT.t[0:64, h, :].rearrange("p (a b x) -> p a b x", a=4, b=2)[:, :, par, :]),
                     reads=[qT], writes=[qz[par]])
        for h in range(4):
            pk = ps[h % 2]
            proj_T(c, pk, lambda kc, h=h: wB.t[:, kc, 256 + h * 64:256 + (h + 1) * 64], h_, wBa, M=64)
            P.op("act", lambda e, h=h, pk=pk: e.activation(kT.t[0:64, h, :], pk.t[0:64, :], AF.Copy), reads=[pk], writes=[kT])
        for t4 in range(4):
            pA, pBk = ps[2], ps[3]
            for kc in range(KC):
                P.op("pe", lambda e, kc=kc, t4=t4, h_=h_: e.matmul(pA.t[:], h_.t[:, kc, t4 * 128:(t4 + 1) * 128], wB.t[:, kc, 256:768], start=(kc == 0), stop=(kc == KC - 1)),
                     reads=[h_, wBa], writes=[pA])
            for kc in range(KC):
                P.op("pe", lambda e, kc=kc, t4=t4, h_=h_: e.matmul(pBk.t[:, 0:264], h_.t[:, kc, t4 * 128:(t4 + 1) * 128], wB.t[:, kc, 768:1032], start=(kc == 0), stop=(kc == KC - 1)),
                     reads=[h_, wBa], writes=[pBk])
            P.op("act", lambda e, t4=t4: e.activation(ktok.t[:, t4, :, :].rearrange("p h x -> p (h x)"), pA.t[:, 0:256], AF.Copy), reads=[pA], writes=[ktok])
            P.op("act", lambda e, t4=t4: e.activation(vtmp.t[:, t4, 0:256], pA.t[:, 256:512], AF.Copy), reads=[pA], writes=[vtmp])
            P.op("dve", lambda e, t4=t4: e.tensor_copy(vtmp.t[:, t4, 256:512], pBk.t[:, 0:256]), reads=[pBk], writes=[vtmp])
            P.op("dve", lambda e, t4=t4: e.tensor_copy(graw.t[:, t4, :], pBk.t[:, 256:264]), reads=[pBk], writes=[graw])

        def og_tiles(tiles):
            for t4 in tiles:
                tc_ = slice(t4 * 128, (t4 + 1) * 128)
                pO, pG = ps[2], ps[3]
                so = so4[t4]
                for kc in range(KC):
                    P.op("pe", lambda e, kc=kc, tc_=tc_, h_=h_: e.matmul(pO.t[:], h_.t[:, kc, tc_], wB.t[:, kc, 1032:1544], start=(kc == 0), stop=(kc == KC - 1)), reads=[h_, wB], writes=[pO])
                for kc in range(KC):
                    P.op("pe", lambda e, kc=kc, tc_=tc_, h_=h_: e.matmul(pG.t[:], h_.t[:, kc, tc_], wB.t[:, kc, 1544:2056], start=(kc == 0), stop=(kc == KC - 1)), reads=[h_, wB], writes=[pG])
                P.op("act", lambda e, so=so: e.activation(so.t[:], pO.t[:], AF.Sigmoid), reads=[pO], writes=[so])
                P.op("act", lambda e: e.activation(sg.t[:], pG.t[:], AF.Silu), reads=[pG], writes=[sg])
                P.op("pool", lambda e, so=so: e.tensor_tensor(so.t[:], so.t[:], sg.t[:], ALU.mult), reads=[so, sg], writes=[so])
                P.op("pool", lambda e, so=so: e.tensor_tensor(so.t[:], so.t[:], gmh.t[:], ALU.mult), reads=[so, gmh], writes=[so])

        P.op("dve", lambda e: e.tensor_tensor(fp.t[:], graw.t[:, :, 4:8], fbb.t[:].unsqueeze(1).to_broadcast([128, 4, 4]), ALU.add), reads=[graw, fbb], writes=[fp])
        P.op("act", lambda e: e.activation(l1.t[:], fp.t[:], AF.Exp, scale=-1.0), reads=[fp], writes=[l1])
        P.op("act", lambda e: e.activation(l1.t[:], l1.t[:], AF.Ln, bias=1.0), reads=[l1], writes=[l1])
        og_tiles((0, 1))
        l1f = l1.t[:].rearrange("p a b -> p (a b)")
        pg = ps[6]
        P.op("pe", lambda e: e.matmul(pg.t[:, 0:16], cf.t[:, C_TRI, :], l1f, start=True, stop=True), reads=[cf, l1], writes=[pg])
        for par in range(2):
            P.op("pe", lambda e, par=par: e.matmul(pg.t[0:64, 16 + par * 16:32 + par * 16], cf.t[:, C_SEL0 + par, 0:64], l1f, start=True, stop=True),
                 reads=[cf, l1], writes=[pg])
        nb3 = pg.t[:, 0:16].rearrange("p (a b) -> p a b", a=4)
        P.op("act", lambda e: e.activation(eb.t[:], nb3, AF.Exp, scale=-1.0), reads=[pg], writes=[eb])
        P.op("dve", lambda e: e.tensor_tensor(ui.t[:], nb3, graw.t[:, :, 0:4], ALU.add), reads=[pg, graw], writes=[ui])
        P.op("act", lambda e: e.activation(uu.t[:], ui.t[:], AF.Exp), reads=[ui], writes=[uu])
        P.op("act", lambda e, eg_cur=eg_cur: e.activation(eg_cur.t[:].rearrange("p a b c -> p (a b c)"), pg.t[0:64, 16:48], AF.Exp, scale=-1.0), reads=[pg], writes=[eg_cur])
        og_tiles((2, 3))
        P.op("dve", lambda e: e.tensor_tensor(vp.t[:, :, :, 0:128], vtmp.t[:].rearrange("p a (h x) -> p a h x", h=4),
                                              uu.t[:].unsqueeze(3).to_broadcast([128, 4, 4, 128]), ALU.mult), reads=[vtmp, uu], writes=[vp])
        P.op("dve", lambda e: e.tensor_copy(vp.t[:, :, :, 128:129], uu.t[:].unsqueeze(3)), reads=[uu], writes=[vp])
        for cidx in range(8):
            t4, par = cidx // 2, cidx % 2
            rs_ = slice(par * 64, par * 64 + 64)
            for hp in range(2):
                pu = ps[hp]
                for hh_ in range(2):
                    h = hp * 2 + hh_
                    P.op("pe", lambda e, h=h, hh_=hh_, t4=t4, rs_=rs_, pu=pu: e.matmul(pu.t[0:64, hh_ * 129:(hh_ + 1) * 129], ktok.t[rs_, t4, h, :], vp.t[rs_, t4, h, 0:129],
                                                                                start=True, stop=True), reads=[ktok, vp], writes=[pu])
                P.op("act", lambda e, hp=hp, cidx=cidx, pu=pu: e.activation(Usb.t[0:64, cidx, 2 * hp:2 * hp + 2, :].rearrange("p h x -> p (h x)"), pu.t[0:64, 0:258], AF.Copy),
                     reads=[pu], writes=[Usb])
        for t4 in range(4):
            tc_ = slice(t4 * 128, (t4 + 1) * 128)
            pS = ps[6] if t4 % 2 == 0 else ps[0]
            stm = STm[t4]
            for h in range(4):
                P.op("pe", lambda e, h=h, tc_=tc_, pS=pS: e.matmul(pS.t[:, h * 128:(h + 1) * 128], kT.t[0:64, h, tc_], qT.t[0:64, h, tc_], start=True, stop=True),
                     reads=[kT, qT], writes=[pS])
            P.op("dve", lambda e, stm=stm, pS=pS: e.tensor_tensor(stm.t[:], pS.t[:].rearrange("p (h t) -> p h t", h=4),
                                                          cf.t[:, C_TRI:C_TRI + 1, :].to_broadcast([128, 4, 128]), ALU.mult), reads=[pS, cf], writes=[stm])
        for cidx in range(8):
            gc = T * 8 + cidx
            if gc == 0:
                P.op("dve", lambda e: e.tensor_copy(Z.t[:], Usb.t[0:64, 0, :, :]), reads=[Usb], writes=[Z])
                continue
            pc = cidx - 1
            if pc >= 0:
                egp = eg_cur.t[0:64, pc % 2, pc // 2, :]
                egb_ = eg_cur
            else:
                egp = eg_prev.t[0:64, 1, 3, :]
                egb_ = eg_prev
            egp3 = egp.unsqueeze(2).to_broadcast([64, 4, 129])
            P.op("pool", lambda e, cidx=cidx, egp3=egp3: e.tensor_tensor(Cb[cidx].t[0:64, :, 0:129], Z.t[:], egp3, ALU.mult), reads=[Z, egb_], writes=[Cb[cidx]])
            P.op("dve", lambda e, egp3=egp3: e.tensor_tensor(Z.t[:], Z.t[:], egp3, ALU.mult), reads=[Z, egb_], writes=[Z])
            P.op("dve", lambda e, cidx=cidx: e.tensor_tensor(Z.t[:], Z.t[:], Usb.t[0:64, cidx, :, :], ALU.add), reads=[Z, Usb], writes=[Z])
        yo_ = yo[T % 2]

        def nd_banks(t4):
            return (ps[4], ps[5]) if t4 % 2 == 0 else (ps[2], ps[3])

        def nd_mm(t4):
            tc_ = slice(t4 * 128, (t4 + 1) * 128)
            stm = STm[t4]
            bk = nd_banks(t4)
            for h in range(4):
                pn = bk[h // 2]
                o0 = (h % 2) * 129
                P.op("pe", lambda e, h=h, t4=t4, pn=pn, o0=o0, stm=stm: e.matmul(pn.t[:, o0:o0 + 129], stm.t[:, h, :], vp.t[:, t4, h, 0:129], start=True, stop=False),
                     reads=[stm, vp], writes=[pn])
                for par in range(2):
                    cb_ = Cb[2 * t4 + par]
                    P.op("pe", lambda e, h=h, par=par, pn=pn, o0=o0, cb_=cb_, tc_=tc_: e.matmul(pn.t[:, o0:o0 + 129], qz[par].t[0:64, h, tc_], cb_.t[0:64, h, 0:129],
                                                                                          start=False, stop=(par == 1)), reads=[qz[par], cb_], writes=[pn])

        def out_chain(t4):
            bk = nd_banks(t4)
            for b2 in range(2):
                pn = bk[b2]
                P.op("dve", lambda e, pn=pn, b2=b2, t4=t4: e.tensor_tensor(dn.t[:, 2 * b2:2 * b2 + 2], pn.t[:, 0:258].rearrange("p (h x) -> p h x", h=2)[:, :, 128],
                                                                        eb.t[:, t4, 2 * b2:2 * b2 + 2], ALU.mult), reads=[pn, eb], writes=[dn])
            P.op("dve", lambda e: e.tensor_scalar(rr.t[:], dn.t[:], -1.0, None, ALU.mult), reads=[dn], writes=[rr])
            P.op("dve", lambda e: e.tensor_tensor(dn.t[:], dn.t[:], rr.t[:], ALU.max), reads=[dn, rr], writes=[dn])
            P.op("dve", lambda e: e.tensor_scalar_max(dn.t[:], dn.t[:], 1.0), reads=[dn], writes=[dn])
            P.op("dve", lambda e: e.reciprocal(dn.t[:], dn.t[:]), reads=[dn], writes=[dn])
            P.op("dve", lambda e, t4=t4: e.tensor_tensor(rr.t[:], eb.t[:, t4, :], dn.t[:], ALU.mult), reads=[eb, dn], writes=[rr])
            for b2 in range(2):
                pn = bk[b2]
                P.op("dve", lambda e, pn=pn, b2=b2: e.tensor_tensor(hh.t[:, 2 * b2:2 * b2 + 2, :], pn.t[:, 0:258].rearrange("p (h x) -> p h x", h=2)[:, :, 0:128],
                                                                  rr.t[:, 2 * b2:2 * b2 + 2].unsqueeze(2).to_broadcast([128, 2, 128]), ALU.mult), reads=[pn, rr], writes=[hh])

        def out_chain2(t4):
            tc_ = slice(t4 * 128, (t4 + 1) * 128)
            P.op("pool", lambda e: e.tensor_tensor(sq.t[:], hh.t[:], hh.t[:], ALU.mult), reads=[hh], writes=[sq])
            P.op("dve", lambda e: e.tensor_reduce(ssm.t[:], sq.t[:], AX.X, ALU.add), reads=[sq], writes=[ssm])
            rstd_from_ss(P, ssm.t[:], ssm.t[:], 128, [ssm], [ssm])
            P.op("dve", lambda e: e.tensor_tensor(hh.t[:], hh.t[:], ssm.t[:].unsqueeze(2).to_broadcast([128, 4, 128]), ALU.mult), reads=[hh, ssm], writes=[hh])
            so = so4[t4]
            yb_ = ybt[t4 % 2]
            P.op("dve", lambda e, yb_=yb_, so=so: e.tensor_tensor(yb_.t[:], hh.t[:].rearrange("p h x -> p (h x)"), so.t[:], ALU.mult), reads=[hh, so], writes=[yb_])
            for ch in range(4):
                P.op("pe", lambda e, ch=ch, yb_=yb_: e.transpose(c.pst.t[:, ch * 128:(ch + 1) * 128], yb_.t[:, ch * 128:(ch + 1) * 128], c.ident.t[:]),
                     reads=[yb_, c.ident], writes=[c.pst])
            P.op("act", lambda e, yo_=yo_, tc_=tc_: e.activation(yo_.t[:, :, tc_], c.pst.t[:, 0:512].rearrange("p (a t) -> p a t", a=4), AF.Copy), reads=[c.pst], writes=[yo_])

        nd_mm(0)
        nd_mm(1)
        for t4 in range(4):
            out_chain(t4)
            if t4 + 2 < 4:
                nd_mm(t4 + 2)
            out_chain2(t4)
        P.dma("sp", c.yT[1].t[:, :, cols], yo_.t[:], reads=[yo_], writes=[c.yb_views[T]], semkey=K(yo_))
    P.release(m0)


def make_consts():
    s = np.arange(128)[:, None]
    t = np.arange(128)[None, :]
    same = (s // 64) == (t // 64)
    cm = np.zeros((NCONST, 128, 128), np.float32)
    cm[C_IDENT] = np.eye(128, dtype=np.float32)
    cm[C_TRI] = ((s <= t) & same)
    cm[C_BLK] = same
    cm[C_TRIL128] = (s <= t)
    cm[C_SEL0] = np.broadcast_to(s < 64, (128, 128))
    cm[C_SEL1] = np.broadcast_to(s >= 64, (128, 128))
    cm[C_ONES] = 1.0
    cm[C_SELDEN] = np.broadcast_to(s == 64, (128, 128))
    half = 16
    inv = (np.float32(10000.0) ** (-np.arange(half, dtype=np.float32) / np.float32(half))).astype(np.float32)
    invf = np.zeros((128, 4), np.float32)
    r = np.arange(128)
    invf[:, 0] = inv[r % 16]
    first = (r % 32) < 16
    invf[:, 1] = np.where(first, -TWO_PI, TWO_PI)
    invf[:, 2] = np.where(first, -np.pi, np.pi)
    invf[:, 3] = np.pi
    return cm, invf


def pack_weights(inp, n_layers, l0=0):
    sl = slice(l0, l0 + n_layers)
    f = lambda k: np.ascontiguousarray(np.asarray(inp[k], np.float32)[sl])
    w_in = f("w_in")
    w_uq = f("w_uq")
    L = n_layers
    w_uq_sw = np.zeros_like(w_uq)
    for h in range(8):
        b0 = h * 96 + 64
        w_uq_sw[:, :, b0:b0 + 16] = w_uq[:, :, b0 + 16:b0 + 32]
        w_uq_sw[:, :, b0 + 16:b0 + 32] = w_uq[:, :, b0:b0 + 16]
    w_kr = np.zeros((L, D, 2, 96), np.float32)
    w_kr[:, :, 0, 64:96] = w_in[:, :, O_KR:O_KR + 32]
    w_kr[:, :, 1, 64:80] = w_in[:, :, O_KR + 16:O_KR + 32]
    w_kr[:, :, 1, 80:96] = w_in[:, :, O_KR:O_KR + 16]
    w_p = np.stack([f("w_pa"), f("w_pb"), f("w_pc"), f("w_pd")], axis=1)
    w_sT = np.ascontiguousarray(np.transpose(f("w_s"), (0, 1, 3, 2)))
    vecs = np.zeros((L, 128, NVEC), np.float32)
    vecs[:, :, V_GPRE:V_GPRE + 8] = f("g_pre").reshape(L, 8, 128).transpose(0, 2, 1)
    wc = f("w_conv").reshape(L, 3, 4, 128)
    vecs[:, :, V_WCONV:V_WCONV + 12] = wc.transpose(0, 3, 2, 1).reshape(L, 128, 12)
    vecs[:, :, V_BCONV:V_BCONV + 4] = f("b_conv").reshape(L, 4, 128).transpose(0, 2, 1)
    vecs[:, :, V_GSV:V_GSV + 4] = f("g_sv").reshape(L, 4, 128).transpose(0, 2, 1)
    vecs[:, :, V_BGATE:V_BGATE + 32] = f("b_gate").reshape(L, 32, 128).transpose(0, 2, 1)
    vecs[:, :, V_GCQ:V_GCQ + 2] = f("g_cq").reshape(L, 2, 128).transpose(0, 2, 1)
    vecs[:, :, V_GCKV:V_GCKV + 1] = f("g_ckv").reshape(L, 1, 128).transpose(0, 2, 1)
    rows = np.zeros((L, NROW, 1024), np.float32)
    rows[:, R_GPOST, :] = f("g_post")
    rows[:, R_GMH, :512] = f("g_mh")
    rows[:, R_BS, :512] = f("b_s").reshape(L, 512)
    rows[:, R_FB, :4] = f("f_bias")
    cm, invf = make_consts()
    return {
        "w_in": w_in, "w_gate": f("w_gate"), "w_uq": w_uq, "w_uq_sw": w_uq_sw, "w_ukv": f("w_ukv"), "w_kr": w_kr,
        "w_p": w_p, "w_out": f("w_out"), "w_sT": w_sT, "vecs": vecs, "rows": rows, "consts": cm, "inv_freq": invf,
    }


STAGE_FNS = (("attn", lambda c: stage_attn(c), "ya_views"), ("mlstm", lambda c: stage_mlstm(c), "yb_views"),
             ("conv", lambda c: stage_conv(c), "yc_views"), ("sgu", lambda c: stage_sgu(c), "yd_views"))

_PROG = {}


def get_prog(n_layers):
    if n_layers not in _PROG:
        _PROG[n_layers] = build_program(n_layers)
    return _PROG[n_layers]


N_LAUNCH_LAYERS = 2


def kernel(**inp):
    x = np.asarray(inp["x"], np.float32)
    pos = np.asarray(inp["positions"], np.int32)
    B = x.shape[0]
    cur = x
    for l0 in range(0, NL, N_LAUNCH_LAYERS):
        nc, _ = get_prog(N_LAUNCH_LAYERS)
        w = pack_weights(inp, N_LAUNCH_LAYERS, l0)
        in_maps = []
        for b in range(B):
            m = dict(w)
            m["x"] = np.ascontiguousarray(cur[b])
            m["pos"] = np.ascontiguousarray(pos[b][None, :])
            in_maps.append(m)
        res = run_bass_kernel_spmd(nc, in_maps, core_ids=list(range(B)))
        cur = np.stack([np.asarray(r["out"], np.float32) for r in res.results], axis=0)
    return cur
```
